# Optimizing a Trainium2 kernel written in Bass

```python
import jax, jax.numpy as jnp
from jax import lax
import numpy as np

D_MODEL = 4096
BATCH = 4
SEQ = 2048
DEPTH = 1

GRID_W = 64
CTX_LEN = 256
HG_HEADS = 16
HG_HEAD_DIM = 128
HG_WIDTH = HG_HEADS * HG_HEAD_DIM
POOL_WIDTH = D_MODEL - HG_WIDTH
POOL_WINDOWS = (2, 4, 8, 16)
POOL_GROUP = POOL_WIDTH // len(POOL_WINDOWS)
MIX_WIDTH = HG_WIDTH + POOL_WIDTH
IN_COLS = 5 * HG_WIDTH + POOL_WIDTH
CTX_STATE_COLS = 3 * HG_WIDTH
CHUNK = 64
N_EXPERTS = 16
EXPERT_FF = D_MODEL // 2
CAPACITY_FACTOR = 2
EPS = 1e-6

kernel_name = 'hybrid_hgrn2_pool_ec_flow_block'


def rms_norm(x, g):
    x32 = x.astype(jnp.float32)
    y = x32 * lax.rsqrt(jnp.mean(x32 * x32, axis=-1, keepdims=True) + EPS)
    return y * g.astype(jnp.float32)


def ada_mod(cvec, w, b):
    mod = jax.nn.silu(cvec.astype(jnp.float32)) @ w + b
    return jnp.split(mod, 6, axis=-1)


def modulate(h, shift, scale):
    return h * (1.0 + scale) + shift


def lower_bounds(lb_param):
    p = jax.nn.softmax(lb_param.astype(jnp.float32), axis=0)
    return jnp.cumsum(p, axis=0)


def forget_gate(f_raw, lb):
    f = lb + (1.0 - lb) * jax.nn.sigmoid(f_raw.astype(jnp.float32))
    return jnp.log(f), 1.0 - f


def to_heads(t):
    return t.reshape(t.shape[0], t.shape[1], HG_HEADS, HG_HEAD_DIM)


def rev(t):
    return jnp.flip(t, axis=1)


def gla_chunk_scan(q, k, v, logf, s0):
    B, L, H, _ = q.shape
    n = L // CHUNK

    def chunks(t):
        return t.reshape(B, n, CHUNK, H, t.shape[-1]).transpose(1, 0, 3, 2, 4)

    mask = jnp.tril(jnp.ones((CHUNK, CHUNK), dtype=bool))[:, :, None]

    def step(S, inp):
        qc, kc, vc, gc = inp
        b = jnp.cumsum(gc, axis=2)
        o_inter = jnp.einsum('bhtk,bhkv->bhtv', qc * jnp.exp(b), S)
        diff = b[:, :, :, None, :] - b[:, :, None, :, :]
        decay = jnp.where(mask, jnp.exp(jnp.where(mask, diff, 0.0)), 0.0)
        scores = jnp.einsum('bhtk,bhsk,bhtsk->bhts', qc, kc, decay)
        o_intra = jnp.einsum('bhts,bhsv->bhtv', scores, vc)
        b_end = b[:, :, -1:, :]
        S_new = (jnp.exp(b_end[:, :, 0, :])[..., None] * S
                 + jnp.einsum('bhsk,bhsv->bhkv', kc * jnp.exp(b_end - b), vc))
        return S_new, o_inter + o_intra

    S_fin, o = lax.scan(step, s0, (chunks(q), chunks(k), chunks(v), chunks(logf)))
    o = o.transpose(1, 0, 3, 2, 4).reshape(B, L, H, v.shape[-1])
    return o, S_fin


def final_state(k, v, logf):
    suffix = jnp.flip(jnp.cumsum(jnp.flip(logf, 1), axis=1), 1) - logf
    return jnp.einsum('blhk,blhv->bhkv', k * jnp.exp(suffix), v)


def hgrn2_gates(f_fwd, f_bwd, lb):
    logf_f, k_f = forget_gate(f_fwd, lb[0])
    logf_b, k_b = forget_gate(f_bwd, lb[1])
    return to_heads(logf_f), to_heads(k_f), to_heads(logf_b), to_heads(k_b)


def hgrn2_context_states(f_fwd, f_bwd, i, lb):
    logf_f, k_f, logf_b, k_b = hgrn2_gates(f_fwd, f_bwd, lb)
    i = to_heads(i)
    s_f = final_state(k_f, i, logf_f)
    s_b = final_state(rev(k_b), rev(i), rev(logf_b))
    return s_f, s_b


def hgrn2_mixer(q, f_fwd, f_bwd, i, g, lb, s0_f, s0_b, norm_g):
    logf_f, k_f, logf_b, k_b = hgrn2_gates(f_fwd, f_bwd, lb)
    q, i = to_heads(q), to_heads(i)
    o_f, s_f = gla_chunk_scan(q, k_f, i, logf_f, s0_f)
    o_b, s_b = gla_chunk_scan(rev(q), rev(k_b), rev(i), rev(logf_b), s0_b)
    o = (o_f + rev(o_b)).astype(jnp.float32)
    o = o * lax.rsqrt(jnp.mean(o * o, axis=-1, keepdims=True) + EPS)
    o = o.reshape(o.shape[0], o.shape[1], HG_WIDTH) * norm_g * jax.nn.silu(g.astype(jnp.float32))
    return o, s_f, s_b


def box_bounds(n, w):
    idx = jnp.arange(n)
    start = idx - w // 2
    return jnp.clip(start, 0, n), jnp.clip(start + w, 0, n)


def grid_window_mean(v, w):
    B, L, C = v.shape
    rows = L // GRID_W
    g = v.reshape(B, rows, GRID_W, C)
    P = jnp.pad(jnp.cumsum(jnp.cumsum(g, axis=1), axis=2), ((0, 0), (1, 0), (1, 0), (0, 0)))
    r0, r1 = box_bounds(rows, w)
    c0, c1 = box_bounds(GRID_W, w)

    def corner(r, cidx):
        return jnp.take(jnp.take(P, r, axis=1), cidx, axis=2)

    s = corner(r1, c1) - corner(r0, c1) - corner(r1, c0) + corner(r0, c0)
    cnt = ((r1 - r0)[:, None] * (c1 - c0)[None, :]).astype(jnp.float32)
    return (s / cnt[None, :, :, None]).reshape(B, L, C)


def seq_window_mean(v, w):
    B, L, C = v.shape
    P = jnp.pad(jnp.cumsum(v, axis=1), ((0, 0), (1, 0), (0, 0)))
    t0, t1 = box_bounds(L, w)
    s = jnp.take(P, t1, axis=1) - jnp.take(P, t0, axis=1)
    return s / (t1 - t0).astype(jnp.float32)[None, :, None]


def multiscale_pool(v, pool_w, pool_scale, on_grid):
    v32 = v.astype(jnp.float32)
    outs = []
    for gi, w in enumerate(POOL_WINDOWS):
        vg = v32[..., gi * POOL_GROUP:(gi + 1) * POOL_GROUP]
        mean = grid_window_mean(vg, w) if on_grid else seq_window_mean(vg, w)
        outs.append((mean - vg) @ pool_w[gi])
    return jnp.concatenate(outs, axis=-1) * pool_scale


def expert_choice_ffn(h, router_w, w1, w3, w2):
    B, L, D = h.shape
    cap = CAPACITY_FACTOR * L // N_EXPERTS
    aff = jax.nn.softmax((h @ router_w).astype(jnp.float32), axis=-1)
    gate, idx = lax.top_k(jnp.swapaxes(aff, 1, 2), cap)
    xg = jax.vmap(lambda hb, ib: hb[ib])(h, idx)
    hid = jax.nn.silu(jnp.einsum('becd,edf->becf', xg, w1)) * jnp.einsum('becd,edf->becf', xg, w3)
    y = jnp.einsum('becf,efd->becd', hid, w2) * gate[..., None]
    out = jax.vmap(lambda ib, yb: jnp.zeros((L, D), yb.dtype).at[ib.reshape(-1)].add(yb.reshape(-1, D)))(idx, y)
    return out


def split_in(p):
    H = HG_WIDTH
    return (p[..., :H], p[..., H:2 * H], p[..., 2 * H:3 * H], p[..., 3 * H:4 * H],
            p[..., 4 * H:5 * H], p[..., 5 * H:])


def setup_inputs(seed: int = 0) -> dict:
    key = jax.random.key(seed)
    ks = jax.random.split(key, 20)
    nrm = jax.random.normal
    D = D_MODEL
    return {
        'x': nrm(ks[0], (BATCH, SEQ, D), jnp.float32),
        'c': nrm(ks[1], (BATCH, D), jnp.float32),
        'ctx': nrm(ks[2], (BATCH, CTX_LEN, D), jnp.float32),
        'c_ctx': nrm(ks[3], (D,), jnp.float32),
        'ada_w': nrm(ks[4], (DEPTH, D, 6 * D), jnp.float32) * (0.5 * D ** -0.5),
        'ada_b': 0.02 * nrm(ks[5], (DEPTH, 6 * D), jnp.float32),
        'norm1_g': 1.0 + 0.05 * nrm(ks[6], (DEPTH, D), jnp.float32),
        'norm2_g': 1.0 + 0.05 * nrm(ks[7], (DEPTH, D), jnp.float32),
        'w_in': nrm(ks[8], (DEPTH, D, IN_COLS), jnp.float32) * D ** -0.5,
        'lb_param': nrm(ks[9], (DEPTH + 1, 2, HG_WIDTH), jnp.float32),
        'hg_norm_g': 1.0 + 0.05 * nrm(ks[10], (DEPTH, HG_WIDTH), jnp.float32),
        'pool_w': nrm(ks[11], (DEPTH, len(POOL_WINDOWS), POOL_GROUP, POOL_GROUP), jnp.float32) * POOL_GROUP ** -0.5,
        'pool_scale': 1.0 + 0.1 * nrm(ks[12], (DEPTH, POOL_WIDTH), jnp.float32),
        'w_out': nrm(ks[13], (DEPTH, MIX_WIDTH, D), jnp.float32) * MIX_WIDTH ** -0.5,
        'router_w': nrm(ks[14], (DEPTH, D, N_EXPERTS), jnp.float32) * D ** -0.5,
        'moe_w1': nrm(ks[15], (DEPTH, N_EXPERTS, D, EXPERT_FF), jnp.float32) * D ** -0.5,
        'moe_w3': nrm(ks[16], (DEPTH, N_EXPERTS, D, EXPERT_FF), jnp.float32) * D ** -0.5,
        'moe_w2': nrm(ks[17], (DEPTH, N_EXPERTS, EXPERT_FF, D), jnp.float32) * EXPERT_FF ** -0.5,
        'final_norm_g': 1.0 + 0.05 * nrm(ks[18], (D,), jnp.float32),
    }


def reference(x, c, ctx, c_ctx, ada_w, ada_b, norm1_g, norm2_g, w_in, lb_param, hg_norm_g,
              pool_w, pool_scale, w_out, router_w, moe_w1, moe_w3, moe_w2, final_norm_g):
    out_dtype = x.dtype
    lbs = lower_bounds(lb_param)
    zero_state = jnp.zeros((ctx.shape[0], HG_HEADS, HG_HEAD_DIM, HG_HEAD_DIM), jnp.float32)
    for layer in range(DEPTH):
        last = layer == DEPTH - 1
        sh1, sc1, g1, sh2, sc2, g2 = ada_mod(c[:, None, :], ada_w[layer], ada_b[layer])
        csh1, csc1, cg1, csh2, csc2, cg2 = ada_mod(c_ctx[None, None, :], ada_w[layer], ada_b[layer])

        hc = modulate(rms_norm(ctx, norm1_g[layer]), csh1, csc1)
        if last:
            pc = hc @ w_in[layer][:, :CTX_STATE_COLS]
            s_f, s_b = hgrn2_context_states(pc[..., :HG_WIDTH], pc[..., HG_WIDTH:2 * HG_WIDTH],
                                            pc[..., 2 * HG_WIDTH:], lbs[layer])
        else:
            cf_f, cf_b, ci, cq, cgo, cv = split_in(hc @ w_in[layer])
            oc, s_f, s_b = hgrn2_mixer(cq, cf_f, cf_b, ci, cgo, lbs[layer], zero_state, zero_state,
                                       hg_norm_g[layer])
            pcm = multiscale_pool(cv, pool_w[layer], pool_scale[layer], on_grid=False)
            ctx_next = ctx + cg1 * (jnp.concatenate([oc, pcm], axis=-1) @ w_out[layer])
            hc2 = modulate(rms_norm(ctx_next, norm2_g[layer]), csh2, csc2)
            ctx_next = ctx_next + cg2 * expert_choice_ffn(hc2, router_w[layer], moe_w1[layer],
                                                          moe_w3[layer], moe_w2[layer])

        h = modulate(rms_norm(x, norm1_g[layer]), sh1, sc1)
        f_f, f_b, i, q, go, v = split_in(h @ w_in[layer])
        o, _, _ = hgrn2_mixer(q, f_f, f_b, i, go, lbs[layer], s_f, s_b, hg_norm_g[layer])
        pm = multiscale_pool(v, pool_w[layer], pool_scale[layer], on_grid=True)
        x = x + g1 * (jnp.concatenate([o, pm], axis=-1) @ w_out[layer])
        h2 = modulate(rms_norm(x, norm2_g[layer]), sh2, sc2)
        x = x + g2 * expert_choice_ffn(h2, router_w[layer], moe_w1[layer], moe_w3[layer], moe_w2[layer])

        if not last:
            ctx = ctx_next
    return rms_norm(x, final_norm_g).astype(out_dtype)
```

```python
import numpy as np
from contextlib import ExitStack
import concourse.bass as bass
import concourse.mybir as mybir
from concourse.bass_utils import run_bass_kernel_spmd

F32 = mybir.dt.float32
BF16 = mybir.dt.bfloat16
I32 = mybir.dt.int32
AF = mybir.ActivationFunctionType
ALU = mybir.AluOpType

D = 4096
L = 2048
CTX = 256
HGW = 2048
NH = 16
INC = 12288
NE = 16
FF = 2048
CAP = 256
EPS = 1e-6
N_CORES = 4

ENGS = ("pe", "act", "dve", "pool", "sp")
N_DMA_SEMS = 12
HEAD_BARRIER = True


class Sched:
    def __init__(self):
        self.ops = {e: [] for e in ENGS}
        self.seq = {e: 0 for e in ENGS}
        self.seen = {e: {} for e in ENGS}
        self.last_w = {}
        self.readers = {}
        self.dma_cnt = [0] * N_DMA_SEMS
        self.dma_rr = 0
        self.out_deps = []

    def _need(self, eng, dep, waits, war=False):
        if dep is None:
            return
        sk, val = dep
        if sk == eng and (eng == "pe" or war):
            return
        if self.seen[eng].get(sk, 0) >= val:
            return
        self.seen[eng][sk] = val
        waits.append((sk, val))

    def alias(self, newkey, oldkeys):
        rs = self.readers.setdefault(newkey, [])
        for k in oldkeys:
            if k in self.last_w and self.last_w[k] is not None:
                rs.append(self.last_w[k])
            rs.extend(self.readers.get(k, ()))

    def op(self, eng, fn, reads=(), writes=(), dma=False, is_out=False):
        waits = []
        for r in reads:
            self._need(eng, self.last_w.get(r), waits)
        for w in writes:
            self._need(eng, self.last_w.get(w), waits)
            for d in self.readers.get(w, ()):
                self._need(eng, d, waits, war=True)
        if dma:
            j = self.dma_rr
            self.dma_rr = (self.dma_rr + 1) % N_DMA_SEMS
            sk = ("dma", j)
            prev = self.dma_cnt[j]
            if prev > 0 and self.seen[eng].get(sk, 0) < prev:
                self.seen[eng][sk] = prev
                waits.append((sk, prev))
            self.dma_cnt[j] = prev + 16
            dep = (sk, prev + 16)
            inc = (sk, 16)
        else:
            self.seq[eng] += 1
            dep = (eng, self.seq[eng])
            inc = (eng, 1)
        self.ops[eng].append((waits, fn, inc))
        for w in writes:
            self.last_w[w] = dep
            self.readers[w] = []
        for r in reads:
            self.readers.setdefault(r, []).append(dep)
        if is_out:
            self.out_deps.append(dep)
        return dep

    def barrier(self):
        for eng in ENGS:
            waits = []
            for e2 in ENGS:
                if e2 != eng and self.seq[e2] > 0:
                    self._need(eng, (e2, self.seq[e2]), waits)
            if eng not in ("pe", "sp") and self.seq[eng] > 0:
                self._need(eng, (eng, self.seq[eng]), waits)
            for j in range(N_DMA_SEMS):
                if self.dma_cnt[j] > 0:
                    self._need(eng, (("dma", j), self.dma_cnt[j]), waits)
            if waits:
                self.ops[eng].append((waits, None, None))

    def finish(self):
        waits = []
        for d in self.out_deps:
            self._need("sp", d, waits)
        if waits:
            self.ops["sp"].append((waits, None, None))

    def emit(self, nc):
        with ExitStack() as es:
            sems = {}
            for e in ENGS:
                sems[e] = es.enter_context(nc.semaphore("s_" + e))
            for j in range(N_DMA_SEMS):
                sems[("dma", j)] = es.enter_context(nc.semaphore("s_dma%d" % j))
            block = es.enter_context(nc.Block())

            def run(engh, ename):
                for waits, fn, inc in self.ops[ename]:
                    for sk, val in waits:
                        engh.wait_ge(sems[sk], val)
                    if fn is None:
                        continue
                    try:
                        ins = fn(engh)
                    except Exception:
                        print("EMIT FAIL", ename, "op#", self.ops[ename].index((waits, fn, inc)), "of", len(self.ops[ename]))
                        raise
                    ins.then_inc(sems[inc[0]], inc[1])

            block.tensor(lambda e: run(e, "pe"))
            block.scalar(lambda e: run(e, "act"))
            block.vector(lambda e: run(e, "dve"))
            block.gpsimd(lambda e: run(e, "pool"))
            block.sync(lambda e: run(e, "sp"))


class Arena:
    def __init__(self, S, big, nbytes):
        self.S = S
        self.big = big
        self.nbytes = nbytes
        self.live = []
        self.cnt = 0

    def carve(self, name, lo, nbytes, dt=F32, parts=128):
        hi = lo + nbytes
        assert hi <= self.nbytes and lo % 4 == 0 and nbytes % 4 == 0, (name, lo, nbytes)
        self.cnt += 1
        key = "%s#%d" % (name, self.cnt)
        keep, old = [], []
        for (a, b, k) in self.live:
            if a < hi and lo < b:
                old.append(k)
                if a < lo:
                    keep.append((a, lo, k))
                if hi < b:
                    keep.append((hi, b, k))
            else:
                keep.append((a, b, k))
        self.S.alias(key, old)
        keep.append((lo, hi, key))
        self.live = keep
        ap = self.big[0:parts, lo // 4:hi // 4]
        if dt != F32:
            ap = ap.bitcast(dt)
        return ap, key


class _Stop(Exception):
    pass


def build_program(upto="all", dbg=False, sa=None, nhb=NH, npool=4, bstop=99):
    nc = bass.Bass("TRN2", target_bir_lowering=False)
    S = Sched()

    SA_KEEP = {"B": {"ident", "lbp", "hgnT", "psclT", "mk", "pmat", "icnt", "pool_w", "projT", "cprojT"}}

    def din(name, shape, dt=F32):
        if sa is not None and name not in SA_KEEP[sa]:
            return nc.dram_tensor(name, [128, 128], dt, kind="Internal").ap()
        return nc.dram_tensor(name, list(shape), dt, kind="ExternalInput").ap()

    def dscr(name, shape, dt=F32):
        return nc.dram_tensor(name, list(shape), dt, kind="Internal").ap()

    def dout(name, shape, dt=F32):
        return nc.dram_tensor(name, list(shape), dt, kind="ExternalOutput").ap()

    x_d = din("x", [L, D])
    ctx_d = din("ctx", [CTX, D])
    c2T_d = din("c2T", [128, 64])
    ada_w_d = din("ada_w", [D, 6 * D])
    ada_bT_d = din("ada_bT", [128, 192])
    n1gT_d = din("n1gT", [128, 32])
    w_in_d = din("w_in", [D, INC])
    ident_d = din("ident", [128, 128])
    lbp_d = din("lbp", [128, 64])
    hgnT_d = din("hgnT", [128, 16])
    psclT_d = din("psclT", [128, 16])
    mk_d = din("mk", [128, 128])
    pmat_d = din("pmat", [128, 19 * 128])
    icnt_d = din("icnt", [4, 128, 2048])
    pool_w_d = din("pool_w", [4, 512, 512])
    mixT_d = dscr("mixT", [D, L], BF16)
    w_out_d = din("w_out", [D, D])
    n2gT_d = din("n2gT", [128, 32])
    fngT_d = din("fngT", [128, 32])
    iota_d = din("iota256", [128, 256])
    tokidx_d = din("tokidx", [128, 16])
    rwT_d = din("rwT", [128, 512])
    w1_d = din("moe_w1", [NE, D, FF])
    w3_d = din("moe_w3", [NE, D, FF])
    w2_d = din("moe_w2", [NE, FF, D])
    x1_d = dscr("x1", [L, D])
    h2_d = dscr("h2", [L, D], BF16)
    moe_d = [dscr("moe0", [L, 2048]), dscr("moe1", [L, 2048])]
    out_d = dout("out", [L, D])
    projT_d = (din if sa == "B" else dscr)("projT", [INC, L])
    cprojT_d = (din if sa == "B" else dscr)("cprojT", [3 * HGW, CTX])
    dbg_out = {}
    if dbg:
        dbg_out["mods"] = dout("d_mods", [128, 384])
        dbg_out["hT"] = dout("d_hT", [128, 32 * 1024], BF16)

    es = ExitStack()
    with es, nc.allow_low_precision("bf16 matmul operands, fp32 accumulation"):
        SB_BYTES = 206 * 1024
        big = es.enter_context(nc.sbuf_tensor("arena", [128, SB_BYTES // 4], F32))
        psum = es.enter_context(nc.psum_tensor("psum", [128, 8 * 512], F32))
        A = Arena(S, big[:], SB_BYTES)
        PS = Arena(S, psum[:], 16 * 1024)

        def bank(name, b, nb=1, dt=F32):
            return PS.carve(name, b * 2048, nb * 2048, dt)

        P0 = 168 * 1024
        ident_f, k_idf = A.carve("ident_f", P0, 512)
        ident_b, k_idb = A.carve("ident_b", P0 + 512, 256, BF16)
        mods, k_mods = A.carve("mods", P0 + 768, 192 * 2 * 4)
        n1gT, k_n1g = A.carve("n1gT", P0 + 768 + 1536, 128)
        A1, k_A1 = A.carve("A1", P0 + 2432, 128)
        A1c, k_A1c = A.carve("A1c", P0 + 2560, 128)
        c2T, k_c2T = A.carve("c2T", P0 + 2688, 256)
        scT, k_scT = A.carve("scT", P0 + 2944, 256)
        adab, k_adab = A.carve("adab", P0 + 3200, 768)
        small, k_small = A.carve("small", P0 + 3968, 256)

        mods3 = mods.rearrange("p (j t) -> p j t", t=2)

        def ld(out_ap, in_ap, key, eng="sp"):
            S.op(eng, lambda e: e.dma_start(out=out_ap, in_=in_ap), writes=[key], dma=True)

        ld(ident_f, ident_d, k_idf)
        if sa is None:
            ld(c2T, c2T_d, k_c2T)
            ld(adab, ada_bT_d, k_adab)
            ld(n1gT, n1gT_d, k_n1g)
        S.op("dve", lambda e: e.tensor_copy(out=ident_b, in_=ident_f), reads=[k_idf], writes=[k_idb])
        if sa is None:
            S.op("act", lambda e: e.activation(out=scT, in_=c2T, func=AF.Silu), reads=[k_c2T], writes=[k_scT])

        if sa is None:
            mod_ps, k_modps = bank("mod_ps", 0)
            NB0 = 3
            wst0 = [A.carve("wst0_%d" % i, i * 16384, 16384) for i in range(NB0)]
            n_ld = 0
            PIECE = 4096
            for pc in range(6):
                for kc in range(32):
                    buf, bkey = wst0[n_ld % NB0]
                    n_ld += 1
                    src = ada_w_d[kc * 128:(kc + 1) * 128, pc * PIECE:(pc + 1) * PIECE]
                    ld(buf, src, bkey)
                    for j in range(32):
                        jj = pc * 32 + j
                        S.op("pe", (lambda e, buf=buf, j=j, jj=jj, kc=kc: e.matmul(
                            mod_ps[:, jj * 2:jj * 2 + 2], buf[:, j * 128:(j + 1) * 128], scT[:, kc * 2:kc * 2 + 2],
                            start=(kc == 0 and jj == 0), stop=(kc == 31), skip_group_check=True)),
                            reads=[bkey, k_scT], writes=[k_modps])
            S.op("dve", lambda e: e.tensor_tensor(out=mods3, in0=mod_ps[:, 0:384].rearrange("p (j t) -> p j t", t=2),
                                                  in1=adab.unsqueeze(2).to_broadcast([128, 192, 2]), op=ALU.add),
                 reads=[k_modps, k_adab], writes=[k_mods])
            S.op("dve", lambda e: e.scalar_tensor_tensor(out=A1, in0=mods3[:, 32:64, 0], scalar=1.0, in1=n1gT,
                                                         op0=ALU.add, op1=ALU.mult), reads=[k_mods, k_n1g], writes=[k_A1])
            S.op("dve", lambda e: e.scalar_tensor_tensor(out=A1c, in0=mods3[:, 32:64, 1], scalar=1.0, in1=n1gT,
                                                         op0=ALU.add, op1=ALU.mult), reads=[k_mods, k_n1g], writes=[k_A1c])
            if dbg:
                S.op("sp", lambda e: e.dma_start(out=dbg_out["mods"], in_=mods), reads=[k_mods], dma=True, is_out=True)

            HT_B = 64 * 1024
            hT, k_hT = None, None

            def norm_transpose(src_d, ntt, hdst, kdst, Acol, Bsel, t_off):
                for g0 in range(0, ntt, 4):
                    gn = min(4, ntt - g0)
                    xs_list = []
                    for t in range(gn):
                        xt, kx = A.carve("xt", HT_B + 16384 + (t % 2) * 16384, 16384)
                        ld(xt, src_d[(g0 + t) * 128:(g0 + t + 1) * 128, :], kx)
                        junk, kj = A.carve("junk", HT_B + 49152, 8192, BF16)
                        ss, kss = A.carve("ss", P0 + 4224 + 48 * t, 48)
                        S.op("pool", lambda e, ss=ss: e.memset(ss, 0.0), writes=[kss])
                        for i8 in range(8):
                            S.op("act", lambda e, xt=xt, junk=junk, ss=ss, i8=i8: e.activation(
                                out=junk[:, i8 * 512:(i8 + 1) * 512], in_=xt[:, i8 * 512:(i8 + 1) * 512], func=AF.Square,
                                accum_out=ss[:, 2 + i8:3 + i8]), reads=[kx, kss], writes=[kj, kss])
                        S.op("dve", lambda e, ss=ss: e.reduce_sum(out=ss[:, 0:1], in_=ss[:, 2:10], axis=mybir.AxisListType.X),
                             reads=[kss], writes=[kss])
                        S.op("dve", lambda e, ss=ss: e.tensor_scalar(out=ss[:, 10:11], in0=ss[:, 0:1], scalar1=1.0 / D,
                                                                     scalar2=EPS, op0=ALU.mult, op1=ALU.add),
                             reads=[kss], writes=[kss])
                        S.op("act", lambda e, ss=ss: e.activation(out=ss[:, 11:12], in_=ss[:, 10:11], func=AF.Sqrt),
                             reads=[kss], writes=[kss])
                        S.op("dve", lambda e, ss=ss: e.reciprocal(out=ss[:, 1:2], in_=ss[:, 11:12]),
                             reads=[kss], writes=[kss])
                        xs, kxs = A.carve("xs", HT_B + 57344 + t * 8192, 8192, BF16)
                        S.op("dve", lambda e, xs=xs, xt=xt, ss=ss: e.tensor_scalar(
                            out=xs, in0=xt, scalar1=ss[:, 1:2], scalar2=None, op0=ALU.mult), reads=[kx, kss], writes=[kxs])
                        xs_list.append((xs, kxs))
                    for kc in range(32):
                        tp, ktp = bank("tp", 4 + (kc % 4), 1, BF16)
                        for t in range(gn):
                            xs, kxs = xs_list[t]
                            S.op("pe", lambda e, tp=tp, xs=xs, t=t, kc=kc: e.transpose(
                                tp[:, t * 128:(t + 1) * 128], xs[:, kc * 128:(kc + 1) * 128], ident_b),
                                reads=[kxs, k_idb], writes=[ktp])
                        dst = hdst[:, kc, t_off + g0 * 128: t_off + (g0 + gn) * 128]
                        if kc % 2 == 0:
                            S.op("dve", lambda e, dst=dst, tp=tp, kc=kc, gn=gn: e.tensor_scalar(
                                out=dst, in0=tp[:, 0:gn * 128], scalar1=Acol[:, kc:kc + 1], scalar2=Bsel[:, kc],
                                op0=ALU.mult, op1=ALU.add), reads=[ktp, k_A1, k_A1c, k_mods], writes=[kdst])
                        else:
                            S.op("act", lambda e, dst=dst, tp=tp, kc=kc, gn=gn: e.activation(
                                out=dst, in_=tp[:, 0:gn * 128], func=AF.Identity, bias=Bsel[:, kc],
                                scale=Acol[:, kc:kc + 1]), reads=[ktp, k_A1, k_A1c, k_mods], writes=[kdst])

            B1 = mods3[:, 0:32, 0:1]
            B1c = mods3[:, 0:32, 1:2]

            hcT_raw, k_hcT = A.carve("hcT", HT_B, 16384, BF16)
            hcT = hcT_raw.rearrange("p (k t) -> p k t", t=CTX)
            hT_raw, k_hT = A.carve("hT", 0, HT_B, BF16)
            hT = hT_raw.rearrange("p (k t) -> p k t", t=1024)

            for hf in range(2):
                if hf == 0:
                    norm_transpose(ctx_d, 2, hcT, k_hcT, A1c, B1c, 0)
                norm_transpose(x_d[hf * 1024:(hf + 1) * 1024, :], 8, hT, k_hT, A1, B1, 0)
                if dbg and hf == 0:
                    S.op("sp", lambda e: e.dma_start(out=dbg_out["hT"], in_=hT_raw), reads=[k_hT], dma=True, is_out=True)
                if upto == "p1":
                    continue
                WA = HT_B + 16384
                for cg in range(96):
                    wst, kws = A.carve("wstA", WA + (cg % 2) * 16384, 16384)
                    for q4 in range(4):
                        S.op("sp", lambda e, wst=wst, cg=cg, q4=q4: e.dma_start(
                            out=wst.rearrange("p (k n) -> p k n", n=128)[:, q4 * 8:(q4 + 1) * 8, :],
                            in_=w_in_d[q4 * 1024:(q4 + 1) * 1024, cg * 128:(cg + 1) * 128].rearrange("(k p) n -> p k n", p=128)),
                            writes=[kws], dma=True)
                    wb, kwb = A.carve("wbA", WA + 32768 + (cg % 3) * 8192, 8192, BF16)
                    S.op("pool", lambda e, wb=wb, wst=wst: e.tensor_copy(out=wb, in_=wst), reads=[kws], writes=[kwb])
                    do_ctx = (hf == 0 and cg < 48)
                    pset = (cg % 2) * 4
                    pbs = [bank("pa", pset + i) for i in range(3 if do_ctx else 2)]
                    for kc in range(32):
                        for tb in range(2):
                            S.op("pe", lambda e, tb=tb, kc=kc, wb=wb, pb=pbs[tb][0]: e.matmul(
                                pb, wb[:, kc * 128:(kc + 1) * 128], hT[:, kc, tb * 512:(tb + 1) * 512],
                                start=(kc == 0), stop=(kc == 31)), reads=[kwb, k_hT], writes=[pbs[tb][1]])
                        if do_ctx:
                            S.op("pe", lambda e, kc=kc, wb=wb, pb=pbs[2][0]: e.matmul(
                                pb[:, 0:CTX], wb[:, kc * 128:(kc + 1) * 128], hcT[:, kc, :],
                                start=(kc == 0), stop=(kc == 31)), reads=[kwb, k_hcT], writes=[pbs[2][1]])
                    ost, kos = A.carve("ostA", WA + 57344 + (cg % 2) * 5120, 5120)
                    S.op("act", lambda e, ost=ost, pb=pbs[0][0]: e.copy(out=ost[:, 0:512], in_=pb),
                         reads=[pbs[0][1]], writes=[kos])
                    S.op("dve", lambda e, ost=ost, pb=pbs[1][0]: e.tensor_copy(out=ost[:, 512:1024], in_=pb),
                         reads=[pbs[1][1]], writes=[kos])
                    S.op("act", lambda e, ost=ost, cg=cg, hf=hf: e.dma_start(
                        out=projT_d[cg * 128:(cg + 1) * 128, hf * 1024:(hf + 1) * 1024], in_=ost[:, 0:1024]),
                        reads=[kos], writes=[("pj", cg)], dma=True)
                    if do_ctx:
                        S.op("dve", lambda e, ost=ost, pb=pbs[2][0]: e.tensor_copy(out=ost[:, 1024:1280], in_=pb[:, 0:CTX]),
                             reads=[pbs[2][1]], writes=[kos])
                        S.op("act", lambda e, ost=ost, cg=cg: e.dma_start(
                            out=cprojT_d[cg * 128:(cg + 1) * 128, :], in_=ost[:, 1024:1280]),
                            reads=[kos], writes=[("cpj", cg)], dma=True)


        AXX = mybir.AxisListType.X
        P1 = P0 + 4608
        lbp, k_lbp = A.carve("lbp", P1, 256)
        lbv, k_lbv = A.carve("lbv", P1 + 256, 128)
        oml, k_oml = A.carve("oml", P1 + 384, 128)
        hgnT, k_hgn = A.carve("hgnT", P1 + 512, 64)
        psclT, k_pscl = A.carve("psclT", P1 + 576, 64)
        mk, k_mk = A.carve("mk", P1 + 1024, 512)
        segm, k_segm = A.carve("segm", P1 + 1536, 8192)
        ones256, k_ones = A.carve("ones256", P1 + 9728, 1024)
        ones_b, k_onesb = A.carve("ones_b", P1 + 10752, 256, BF16)
        S0f, k_S0f = A.carve("S0f", P1 + 11008, 512)
        S0b, k_S0b = A.carve("S0b", P1 + 11520, 512)
        dec, k_dec = A.carve("dec", P1 + 12032, 128)
        decc, k_decc = A.carve("decc", P1 + 12160, 128)
        pmat, k_pmat = A.carve("pmat", P1 + 12288, 19 * 256, BF16)
        pmat_end = P1 + 12288 + 19 * 256

        def OP(eng, fn, reads, writes):
            S.op(eng, fn, reads=reads, writes=writes)

        def init_B_consts():
            ld(lbp, lbp_d, k_lbp)
            ld(hgnT, hgnT_d, k_hgn)
            ld(psclT, psclT_d, k_pscl)
            ld(mk, mk_d, k_mk)
            OP("pool", lambda e: e.memset(segm, 1.0), [], [k_segm])
            OP("pool", lambda e: e.memset(segm.rearrange("p (c j) -> p c j", j=64)[:, :, 0:1], 0.0), [], [k_segm])
            OP("pool", lambda e: e.memset(ones256, 1.0), [], [k_ones])
            OP("pool", lambda e: e.memset(ones_b, 1.0), [], [k_onesb])
            OP("dve", lambda e: e.tensor_tensor(out=oml, in0=lbp[:, 32:64], in1=lbp[:, 0:32], op=ALU.subtract), [k_lbp], [k_oml])
            OP("act", lambda e: e.activation(out=oml, in_=oml, func=AF.Exp), [k_oml], [k_oml])
            OP("dve", lambda e: e.tensor_scalar(out=oml, in0=oml, scalar1=1.0, scalar2=None, op0=ALU.add), [k_oml], [k_oml])
            OP("dve", lambda e: e.reciprocal(out=lbv, in_=oml), [k_oml], [k_lbv])
            OP("dve", lambda e: e.tensor_scalar(out=oml, in0=lbv, scalar1=-1.0, scalar2=1.0, op0=ALU.mult, op1=ALU.add), [k_lbv], [k_oml])


        def sigmoid_gate_f(dst, src, col, kdst, ksrc, eng2="dve"):
            OP("act", lambda e: e.activation(out=dst, in_=src, func=AF.Exp, scale=-1.0), [ksrc], [kdst])
            OP("dve", lambda e: e.tensor_scalar(out=dst, in0=dst, scalar1=1.0, scalar2=None, op0=ALU.add), [kdst], [kdst])
            OP("dve", lambda e: e.reciprocal(out=dst, in_=dst), [kdst], [kdst])
            OP("dve", lambda e: e.tensor_scalar(out=dst, in0=dst, scalar1=oml[:, col:col + 1], scalar2=lbv[:, col:col + 1],
                                                op0=ALU.mult, op1=ALU.add), [kdst, k_oml, k_lbv], [kdst])

        def transpose_to_tok(srcT, ksrc, dst_tok, kdst, ntiles, bank0):
            for g0 in range(0, ntiles, 4):
                gn = min(4, ntiles - g0)
                tp, ktp = bank("tpB", bank0 + (g0 // 4) % 2, 1, BF16)
                for t in range(gn):
                    OP("pe", lambda e, tp=tp, t=t, g0=g0: e.transpose(
                        tp[:, t * 128:(t + 1) * 128], srcT[:, (g0 + t) * 128:(g0 + t + 1) * 128], ident_b),
                        [ksrc, k_idb], [ktp])
                OP("act", lambda e, tp=tp, g0=g0, gn=gn: e.copy(
                    out=dst_tok[:, g0 * 128:(g0 + gn) * 128], in_=tp[:, 0:gn * 128]), [ktp], [kdst])

        def transpose_to_tok64(srcT, ksrc, dst_tok, kdst, nch, bank0):
            for g0 in range(0, nch, 8):
                gn = min(8, nch - g0)
                tp, ktp = bank("tpB64", bank0 + (g0 // 8) % 2, 1, BF16)
                for t in range(gn):
                    OP("pe", lambda e, tp=tp, t=t, g0=g0: e.transpose(
                        tp[0:64, t * 128:(t + 1) * 128], srcT[:, (g0 + t) * 64:(g0 + t + 1) * 64], ident_b),
                        [ksrc, k_idb], [ktp])
                OP("act", lambda e, tp=tp, g0=g0, gn=gn: e.copy(
                    out=dst_tok[0:64, g0 * 128:(g0 + gn) * 128], in_=tp[0:64, 0:gn * 128]), [ktp], [kdst])

        def phase_B_heads():
            K = 1024
            for h in range(nhb):
                ff, k_ff = A.carve("ff", 0, 8192)
                fb, k_fb = A.carve("fb", 8 * K, 8192)
                iT, k_iT = A.carve("iT", 16 * K, 8192)
                qT, k_qT = A.carve("qT", 24 * K, 8192)
                gT, k_gT = A.carve("gT", 32 * K, 8192)
                for (buf, kb, cg) in ((ff, k_ff, h), (fb, k_fb, 16 + h), (iT, k_iT, 32 + h), (qT, k_qT, 48 + h), (gT, k_gT, 64 + h)):
                    S.op("sp", lambda e, buf=buf, cg=cg: e.dma_start(out=buf, in_=projT_d[cg * 128:(cg + 1) * 128, :]),
                         reads=[("pj", cg)], writes=[kb], dma=True)
                cin, k_cin = A.carve("cin", 40 * K, 3072)
                for j, cg in enumerate((h, 16 + h, 32 + h)):
                    S.op("sp", lambda e, j=j, cg=cg: e.dma_start(out=cin[:, j * 256:(j + 1) * 256], in_=cprojT_d[cg * 128:(cg + 1) * 128, :]),
                         reads=[("cpj", cg)], writes=[k_cin], dma=True)
                if bstop == 1:
                    raise _Stop()
                if dbg and h == 0 and upto == "B":
                    dbg_out["din"] = dout("d_in", [128, 5 * 2048])
                    dbg_out["dsegm"] = dout("d_segm", [128, 2048])
                    for j, (buf, kb) in enumerate(((ff, k_ff), (fb, k_fb), (iT, k_iT), (qT, k_qT), (gT, k_gT))):
                        S.op("sp", lambda e, buf=buf, j=j: e.dma_start(out=dbg_out["din"][:, j * 2048:(j + 1) * 2048], in_=buf),
                             reads=[kb], dma=True, is_out=True)
                    S.op("sp", lambda e: e.dma_start(out=dbg_out["dsegm"], in_=segm), reads=[k_segm], dma=True, is_out=True)
                T1, k_T1 = A.carve("T1", 44 * K, 8192)
                T2, k_T2 = A.carve("T2", 52 * K, 8192)
                T3, k_T3 = A.carve("T3", 60 * K, 8192)
                cb16, k_cb16 = A.carve("cb16", 84 * K, 3 * 512, BF16)
                ctok, k_ctok = A.carve("ctok", 88 * K, 3 * 512, BF16)
                for d in range(2):
                    col = d * 16 + h
                    X = cin[:, d * 256:(d + 1) * 256]
                    f_ = T1[:, d * 256:(d + 1) * 256]
                    g_ = T2[:, d * 256:(d + 1) * 256]
                    cs = T3[:, d * 256:(d + 1) * 256]
                    sigmoid_gate_f(f_, X, col, k_T1, k_cin)
                    OP("act", lambda e, f_=f_, g_=g_: e.activation(out=g_, in_=f_, func=AF.Ln), [k_T1], [k_T2])
                    OP("dve", lambda e, cs=cs, g_=g_: e.tensor_tensor_scan(out=cs, data0=ones256, data1=g_, initial=0.0,
                                                                            op0=ALU.mult, op1=ALU.add), [k_T2, k_ones], [k_T3])
                    if d == 0:
                        OP("dve", lambda e, cs=cs: e.tensor_copy(out=small[:, 0:1], in_=cs[:, 255:256]), [k_T3], [k_small])
                        OP("dve", lambda e, cs=cs: e.tensor_scalar(out=cs, in0=cs, scalar1=-1.0, scalar2=small[:, 0:1],
                                                                     op0=ALU.mult, op1=ALU.add), [k_T3, k_small], [k_T3])
                    else:
                        OP("dve", lambda e, cs=cs, g_=g_: e.tensor_tensor(out=cs, in0=cs, in1=g_, op=ALU.subtract), [k_T3, k_T2], [k_T3])
                    OP("act", lambda e, cs=cs: e.activation(out=cs, in_=cs, func=AF.Exp), [k_T3], [k_T3])
                    OP("dve", lambda e, f_=f_: e.tensor_scalar(out=f_, in0=f_, scalar1=-1.0, scalar2=1.0, op0=ALU.mult, op1=ALU.add), [k_T1], [k_T1])
                    OP("dve", lambda e, f_=f_, cs=cs, d=d: e.tensor_tensor(out=cb16[:, d * 256:(d + 1) * 256], in0=f_, in1=cs, op=ALU.mult),
                       [k_T1, k_T3], [k_cb16])
                OP("act", lambda e: e.copy(out=cb16[:, 512:768], in_=cin[:, 512:768]), [k_cin], [k_cb16])
                transpose_to_tok(cb16, k_cb16, ctok, k_ctok, 6, 6)
                for d, (S0, kS0) in enumerate(((S0f, k_S0f), (S0b, k_S0b))):
                    sp_, ksp = bank("s0ps", 5)
                    for t in range(2):
                        OP("pe", lambda e, sp_=sp_, d=d, t=t: e.matmul(
                            sp_[:, 0:128], ctok[:, (d * 2 + t) * 128:(d * 2 + t + 1) * 128], ctok[:, (4 + t) * 128:(5 + t) * 128],
                            start=(t == 0), stop=(t == 1)), [k_ctok], [ksp])
                    OP("dve", lambda e, sp_=sp_, S0=S0: e.tensor_copy(out=S0, in_=sp_[:, 0:128]), [ksp], [kS0])
                if bstop == 2:
                    raise _Stop()

                Vb, k_Vb = A.carve("Vb", 112 * K, 4096, BF16)
                Vtok, k_Vtok = A.carve("Vtok", 104 * K, 8192, BF16)
                OP("act", lambda e: e.copy(out=Vb, in_=iT), [k_iT], [k_Vb])
                transpose_to_tok64(Vb, k_Vb, Vtok, k_Vtok, 32, 6)
                if bstop == 3:
                    raise _Stop()
                keep = {}
                for d in range(2):
                    col = d * 16 + h
                    X, kX = (ff, k_ff) if d == 0 else (fb, k_fb)
                    T1, k_T1 = A.carve("T1", 44 * K, 8192)
                    T2, k_T2 = A.carve("T2", 52 * K, 8192)
                    T3, k_T3 = A.carve("T3", 60 * K, 8192)
                    kf, k_kf = A.carve("kf", 68 * K, 8192)
                    cu, k_cu = A.carve("cu", 76 * K, 8192)
                    Qt, k_Qt = A.carve("Qt", 84 * K, 4096, BF16)
                    Kt, k_Kt = A.carve("Kt", 88 * K, 4096, BF16)
                    Kh, k_Kh = A.carve("Kh", 92 * K, 4096, BF16)
                    Khtok, k_Khtok = A.carve("Khtok", 96 * K, 8192, BF16)
                    Qh, k_Qh = A.carve("Qh", (132 + 4 * d) * K, 4096, BF16)
                    ATm, k_ATm = A.carve("ATm", (140 + 4 * d) * K, 4096, BF16)
                    Sb, k_Sb = A.carve("Sb", (148 + 8 * d) * K, 8192, BF16)
                    cu3 = cu.rearrange("p (c j) -> p c j", j=64)
                    sigmoid_gate_f(T1, X, col, k_T1, kX)
                    OP("act", lambda e, T1=T1, T2=T2: e.activation(out=T2, in_=T1, func=AF.Ln), [k_T1], [k_T2])
                    OP("pool", lambda e, T1=T1, kf=kf: e.tensor_scalar(out=kf, in0=T1, scalar1=-1.0, scalar2=1.0, op0=ALU.mult, op1=ALU.add), [k_T1], [k_kf])
                    OP("dve", lambda e, cu=cu, T2=T2: e.tensor_tensor_scan(out=cu, data0=segm, data1=T2, initial=0.0, op0=ALU.mult, op1=ALU.add),
                       [k_T2, k_segm], [k_cu])
                    if dbg and h == 0 and upto == "B":
                        dbg_out["dcu%d" % d] = dout("d_cu%d" % d, [128, 2048])
                        dbg_out["dg%d" % d] = dout("d_g%d" % d, [128, 2048])
                        S.op("sp", lambda e, cu=cu, d=d: e.dma_start(out=dbg_out["dcu%d" % d], in_=cu), reads=[k_cu], dma=True, is_out=True)
                        S.op("sp", lambda e, T2=T2, d=d: e.dma_start(out=dbg_out["dg%d" % d], in_=T2), reads=[k_T2], dma=True, is_out=True)
                    OP("dve", lambda e, cu3=cu3: e.tensor_copy(out=small[:, 32:64], in_=cu3[:, :, 63]), [k_cu], [k_small])
                    OP("act", lambda e: e.activation(out=dec, in_=small[:, 32:64], func=AF.Exp), [k_small], [k_dec])
                    if d == 1:
                        OP("dve", lambda e, T1=T1, cu=cu, T2=T2: e.tensor_tensor(out=T1, in0=T2, in1=cu, op=ALU.subtract), [k_T2, k_cu], [k_T1])
                        OP("dve", lambda e, T1=T1, cu=cu: e.tensor_tensor(
                            out=cu.rearrange("p (c j) -> p c j", j=64), in0=T1.rearrange("p (c j) -> p c j", j=64),
                            in1=small[:, 32:64].unsqueeze(2).to_broadcast([128, 32, 64]), op=ALU.add), [k_T1, k_small], [k_cu])
                    endi = 63 if d == 0 else 0
                    midi = 31 if d == 0 else 32
                    OP("act", lambda e, T3=T3, cu=cu: e.activation(out=T3, in_=cu, func=AF.Exp), [k_cu], [k_T3])
                    OP("pool", lambda e, Qh=Qh, T3=T3: e.tensor_tensor(out=Qh, in0=qT, in1=T3, op=ALU.mult), [k_qT, k_T3], [k_Qh])
                    OP("dve", lambda e, T1=T1, cu3=cu3, midi=midi: e.tensor_tensor(
                        out=T1.rearrange("p (c j) -> p c j", j=64), in0=cu3, in1=cu3[:, :, midi:midi + 1].to_broadcast([128, 32, 64]),
                        op=ALU.subtract), [k_cu], [k_T1])
                    OP("act", lambda e, T3=T3, T1=T1: e.activation(out=T3, in_=T1, func=AF.Exp), [k_T1], [k_T3])
                    OP("pool", lambda e, Qt=Qt, T3=T3: e.tensor_tensor(out=Qt, in0=qT, in1=T3, op=ALU.mult), [k_qT, k_T3], [k_Qt])
                    OP("act", lambda e, T2=T2, T1=T1: e.activation(out=T2, in_=T1, func=AF.Exp, scale=-1.0), [k_T1], [k_T2])
                    OP("pool", lambda e, Kt=Kt, T2=T2, kf=kf: e.tensor_tensor(out=Kt, in0=kf, in1=T2, op=ALU.mult), [k_kf, k_T2], [k_Kt])
                    OP("dve", lambda e, T1=T1, cu3=cu3, endi=endi: e.tensor_tensor(
                        out=T1.rearrange("p (c j) -> p c j", j=64), in0=cu3[:, :, endi:endi + 1].to_broadcast([128, 32, 64]), in1=cu3,
                        op=ALU.subtract), [k_cu], [k_T1])
                    OP("act", lambda e, T3=T3, T1=T1: e.activation(out=T3, in_=T1, func=AF.Exp), [k_T1], [k_T3])
                    OP("dve", lambda e, Kh=Kh, T3=T3, kf=kf: e.tensor_tensor(out=Kh, in0=kf, in1=T3, op=ALU.mult), [k_kf, k_T3], [k_Kh])
                    if bstop == 4:
                        raise _Stop()
                    transpose_to_tok64(Kh, k_Kh, Khtok, k_Khtok, 32, 6)
                    if bstop == 5:
                        raise _Stop()
                    Ubuf, k_U = A.carve("Ubuf", 116 * K, 16384)
                    for c0 in range(0, 32, 4):
                        up, kup = bank("ups", (c0 // 4) % 2 + 4)
                        for c in range(c0, c0 + 4):
                            OP("pe", lambda e, up=up, c=c, c0=c0: e.matmul(
                                up[:, (c - c0) * 128:(c - c0 + 1) * 128],
                                Khtok[0:64, c * 128:(c + 1) * 128], Vtok[0:64, c * 128:(c + 1) * 128],
                                start=True, stop=True), [k_Khtok, k_Vtok], [kup])
                        if d == 0:
                            OP("act", lambda e, up=up, c0=c0: e.copy(out=Ubuf[:, c0 * 128:(c0 + 4) * 128], in_=up), [kup], [k_U])
                        else:
                            for c in range(c0, c0 + 4):
                                OP("act", lambda e, up=up, c=c, c0=c0: e.copy(
                                    out=Ubuf[:, (31 - c) * 128:(32 - c) * 128], in_=up[:, (c - c0) * 128:(c - c0 + 1) * 128]), [kup], [k_U])
                    if bstop == 6:
                        raise _Stop()
                    S0, kS0 = (S0f, k_S0f) if d == 0 else (S0b, k_S0b)
                    if d == 0:
                        OP("dve", lambda e: e.tensor_copy(out=decc, in_=dec), [k_dec], [k_decc])
                    else:
                        for c in range(32):
                            OP("pool", lambda e, c=c: e.tensor_copy(out=decc[:, 31 - c:32 - c], in_=dec[:, c:c + 1]), [k_dec], [k_decc])
                    SA0, k_SA0 = A.carve("SallLo", 0, 8192)
                    SA1, k_SA1 = A.carve("SallHi", 16 * K, 8192)

                    def sl(st):
                        return (SA0, k_SA0, st) if st < 16 else (SA1, k_SA1, st - 16)
                    for st in range(32):
                        buf, kbuf, j = sl(st)
                        if st == 0:
                            prev, kprev = S0, kS0
                        else:
                            pb, kpb, pj = sl(st - 1)
                            prev, kprev = pb[:, pj * 128:(pj + 1) * 128], kpb
                        OP("dve", lambda e, st=st, prev=prev, buf=buf, j=j, Ubuf=Ubuf: e.scalar_tensor_tensor(
                            out=buf[:, j * 128:(j + 1) * 128], in0=prev, scalar=decc[:, st:st + 1], in1=Ubuf[:, st * 128:(st + 1) * 128],
                            op0=ALU.mult, op1=ALU.add), [kprev, k_decc, k_U, kbuf], [kbuf])
                    OP("act", lambda e, Sb=Sb, S0=S0: e.copy(out=Sb[:, 0:128], in_=S0), [kS0], [k_Sb])
                    OP("act", lambda e, Sb=Sb, SA0=SA0: e.copy(out=Sb[:, 128:17 * 128], in_=SA0), [k_SA0], [k_Sb])
                    OP("act", lambda e, Sb=Sb, SA1=SA1: e.copy(out=Sb[:, 17 * 128:32 * 128], in_=SA1[:, 0:15 * 128]), [k_SA1], [k_Sb])
                    if bstop == 7:
                        raise _Stop()
                    AT3 = ATm.rearrange("p (c t) -> p c t", t=64)
                    KtZ, k_KtZ = A.carve("KtZ", 60 * K, 4096, BF16)
                    OP("pool", lambda e, KtZ=KtZ, Kt=Kt: e.tensor_copy(out=KtZ, in_=Kt), [k_Kt], [k_KtZ])
                    zlo = 32 if d == 0 else 0
                    OP("pool", lambda e, KtZ=KtZ, zlo=zlo: e.memset(KtZ.rearrange("p (c j) -> p c j", j=64)[:, :, zlo:zlo + 32], 0.0), [], [k_KtZ])
                    for c0 in range(0, 32, 8):
                        ap_, kap = bank("atps", (c0 // 8) % 2 + 6)
                        for c in range(c0, c0 + 8):
                            KL, kKL = (KtZ, k_KtZ) if d == 0 else (Kt, k_Kt)
                            KR, kKR = (Kt, k_Kt) if d == 0 else (KtZ, k_KtZ)
                            OP("pe", lambda e, ap_=ap_, c=c, c0=c0, KL=KL, Qt=Qt: e.matmul(
                                ap_[0:64, (c - c0) * 64:(c - c0) * 64 + 32], KL[:, c * 64:(c + 1) * 64], Qt[:, c * 64:c * 64 + 32],
                                start=True, stop=True), [kKL, k_Qt], [kap])
                            OP("pe", lambda e, ap_=ap_, c=c, c0=c0, KR=KR, Qt=Qt: e.matmul(
                                ap_[0:64, (c - c0) * 64 + 32:(c - c0 + 1) * 64], KR[:, c * 64:(c + 1) * 64], Qt[:, c * 64 + 32:(c + 1) * 64],
                                start=True, stop=True), [kKR, k_Qt], [kap])
                        ap3 = ap_.rearrange("p (c t) -> p c t", t=64)
                        OP("dve", lambda e, ap3=ap3, AT3=AT3, c0=c0, d=d: e.tensor_tensor(
                            out=AT3[0:64, c0:c0 + 8, :], in0=ap3[0:64, :, :],
                            in1=mk[0:64, d * 64:(d + 1) * 64].unsqueeze(1).to_broadcast([64, 8, 64]), op=ALU.mult),
                            [kap, k_mk], [k_ATm])
                    keep[d] = (Qh, k_Qh, ATm, k_ATm, Sb, k_Sb)
                if bstop == 8:
                    raise _Stop()
                sg, k_sg = A.carve("sg", 60 * K, 8192)
                OP("act", lambda e: e.activation(out=sg, in_=gT, func=AF.Exp, scale=-1.0), [k_gT], [k_sg])
                OP("dve", lambda e: e.tensor_scalar(out=sg, in0=sg, scalar1=1.0, scalar2=None, op0=ALU.add), [k_sg], [k_sg])
                OP("dve", lambda e: e.reciprocal(out=sg, in_=sg), [k_sg], [k_sg])
                sgb, k_sgb = A.carve("sgb", 164 * K, 4096, BF16)
                OP("pool", lambda e: e.tensor_tensor(out=sgb, in0=sg, in1=gT, op=ALU.mult), [k_sg, k_gT], [k_sgb])
                mixo, k_mixo = A.carve("mixo", 112 * K, 4096, BF16)
                for tb in range(4):
                    op_, kop = bank("ops", tb % 2)
                    for cl in range(8):
                        c = tb * 8 + cl
                        tile_, r0 = c // 2, (c % 2) * 64
                        n = 0
                        for d in range(2):
                            Qh, k_Qh, ATm, k_ATm, Sb, k_Sb = keep[d]
                            s = c if d == 0 else 31 - c
                            OP("pe", lambda e, op_=op_, cl=cl, ATm=ATm, c=c, n=n: e.matmul(
                                op_[:, cl * 64:(cl + 1) * 64], Vtok[0:64, c * 128:(c + 1) * 128],
                                ATm[0:64, c * 64:(c + 1) * 64], start=(n == 0), stop=False), [k_Vtok, k_ATm], [kop])
                            n += 1
                            OP("pe", lambda e, op_=op_, cl=cl, Sb=Sb, s=s, Qh=Qh, c=c, n=n: e.matmul(
                                op_[:, cl * 64:(cl + 1) * 64], Sb[:, s * 128:(s + 1) * 128], Qh[:, c * 64:(c + 1) * 64],
                                start=False, stop=(n == 3)), [k_Sb, k_Qh], [kop])
                            n += 1
                    sq, k_sq = A.carve("sq", 116 * K + (tb % 2) * 1024, 1024, BF16)
                    OP("act", lambda e, sq=sq, op_=op_: e.activation(out=sq, in_=op_, func=AF.Square), [kop], [k_sq])
                    np_, knp = bank("nps", 2 + tb % 2)
                    OP("pe", lambda e, np_=np_, sq=sq: e.matmul(np_, ones_b, sq, start=True, stop=True), [k_onesb, k_sq], [knp])
                    rs, k_rs = A.carve("rs", 120 * K + (tb % 2) * 2048, 2048)
                    OP("dve", lambda e, rs=rs, np_=np_: e.tensor_scalar(out=rs, in0=np_, scalar1=1.0 / 128, scalar2=EPS, op0=ALU.mult, op1=ALU.add),
                       [knp], [k_rs])
                    OP("act", lambda e, rs=rs: e.activation(out=rs, in_=rs, func=AF.Sqrt), [k_rs], [k_rs])
                    OP("dve", lambda e, rs=rs: e.reciprocal(out=rs, in_=rs), [k_rs], [k_rs])
                    OP("dve", lambda e, rs=rs, op_=op_: e.tensor_tensor(out=rs, in0=op_, in1=rs, op=ALU.mult), [kop, k_rs], [k_rs])
                    OP("dve", lambda e, rs=rs, tb=tb, h=h: e.scalar_tensor_tensor(
                        out=mixo[:, tb * 512:(tb + 1) * 512], in0=rs, scalar=hgnT[:, h:h + 1], in1=sgb[:, tb * 512:(tb + 1) * 512],
                        op0=ALU.mult, op1=ALU.mult), [k_rs, k_hgn, k_sgb], [k_mixo])
                S.op("act", lambda e, h=h: e.dma_start(out=mixT_d[h * 128:(h + 1) * 128, :], in_=mixo),
                     reads=[k_mixo], writes=[("mx", h)], dma=True)
                if dbg and h == 0 and upto == "B":
                    dbg_out["dmixo"] = dout("d_mixo", [128, 2048], BF16)
                    S.op("act", lambda e: e.dma_start(out=dbg_out["dmixo"], in_=mixo), reads=[k_mixo], dma=True, is_out=True)
                if HEAD_BARRIER:
                    S.barrier()

        def phase_B_pools():
            K = 1024
            pst, k_pst = A.carve("pst", 0, 19 * 512)
            ld(pst, pmat_d, k_pst)
            OP("pool", lambda e: e.tensor_copy(out=pmat, in_=pst), [k_pst], [k_pmat])
            OFFS = {0: (-1, 0), 1: (-1, 0, 1), 2: (-2, -1, 0, 1, 2), 3: (-4, -3, -2, -1, 0, 1, 2, 3, 4)}
            mbase = {0: 0, 1: 2, 2: 5, 3: 10}
            for gi in range(npool):
                S.barrier()
                vT, k_vT = A.carve("vT", 0, 32768)
                vb, k_vb = A.carve("vb", 32 * K, 16384, BF16)
                vtok, k_vtok = A.carve("vtok", 48 * K, 16384, BF16)
                icb, k_icb = A.carve("icb", 64 * K, 8192)
                dif, k_dif = A.carve("dif", 72 * K, 16384, BF16)
                pws, k_pws = A.carve("pws", 88 * K, 8192)
                pwb, k_pwb = A.carve("pwb", 96 * K, 4096, BF16)
                ld(icb, icnt_d[gi], k_icb)
                S.op("sp", lambda e, gi=gi: e.dma_start(out=pws.rearrange("p (j n) -> p j n", n=512),
                                                         in_=pool_w_d[gi].rearrange("(j p) n -> p j n", p=128)), writes=[k_pws], dma=True)
                OP("pool", lambda e: e.tensor_copy(out=pwb, in_=pws), [k_pws], [k_pwb])
                for j in range(4):
                    cg = 80 + gi * 4 + j
                    S.op("sp", lambda e, j=j, cg=cg: e.dma_start(out=vT[:, j * 2048:(j + 1) * 2048], in_=projT_d[cg * 128:(cg + 1) * 128, :]),
                         reads=[("pj", cg)], writes=[k_vT], dma=True)
                OP("act", lambda e: e.copy(out=vb, in_=vT), [k_vT], [k_vb])
                for j in range(4):
                    transpose_to_tok(vb[:, j * 2048:(j + 1) * 2048], k_vb, vtok[:, j * 2048:(j + 1) * 2048], k_vtok, 16, 6)
                for j in range(4):
                    for tb in range(4):
                        dp, kdp = bank("dps", (j * 4 + tb) % 2 + 4)
                        for tl in range(4):
                            T_ = tb * 4 + tl
                            offs = [o for o in OFFS[gi] if 0 <= T_ + o <= 15]
                            for n, o in enumerate(offs):
                                mi = mbase[gi] + OFFS[gi].index(o)
                                OP("pe", lambda e, dp=dp, tl=tl, j=j, T_=T_, o=o, mi=mi, n=n, offs=offs: e.matmul(
                                    dp[:, tl * 128:(tl + 1) * 128], vtok[:, j * 2048 + (T_ + o) * 128: j * 2048 + (T_ + o + 1) * 128],
                                    pmat[:, mi * 128:(mi + 1) * 128], start=(n == 0), stop=(n == len(offs) - 1)), [k_vtok, k_pmat], [kdp])
                        tmp, k_tmp = A.carve("ptmp", 100 * K + ((j * 4 + tb) % 2) * 2048, 2048)
                        OP("dve", lambda e, tmp=tmp, dp=dp, tb=tb: e.tensor_tensor(out=tmp, in0=dp, in1=icb[:, tb * 512:(tb + 1) * 512], op=ALU.mult),
                           [kdp, k_icb], [k_tmp])
                        OP("dve", lambda e, tmp=tmp, j=j, tb=tb: e.tensor_tensor(
                            out=dif[:, j * 2048 + tb * 512: j * 2048 + (tb + 1) * 512], in0=tmp,
                            in1=vT[:, j * 2048 + tb * 512: j * 2048 + (tb + 1) * 512], op=ALU.subtract), [k_tmp, k_vT], [k_dif])
                for jo in range(4):
                    mixo, k_mixo = A.carve("mixo", 104 * K + (jo % 2) * 4096, 4096, BF16)
                    for tb in range(4):
                        pp, kpp = bank("pps", (jo * 4 + tb) % 2 + 2)
                        for j in range(4):
                            OP("pe", lambda e, pp=pp, j=j, jo=jo, tb=tb: e.matmul(
                                pp, pwb[:, j * 512 + jo * 128: j * 512 + (jo + 1) * 128],
                                dif[:, j * 2048 + tb * 512: j * 2048 + (tb + 1) * 512], start=(j == 0), stop=(j == 3)), [k_pwb, k_dif], [kpp])
                        OP("act", lambda e, pp=pp, mixo=mixo, tb=tb, gi=gi, jo=jo: e.activation(
                            out=mixo[:, tb * 512:(tb + 1) * 512], in_=pp, func=AF.Identity, scale=psclT[:, gi * 4 + jo: gi * 4 + jo + 1]),
                            [kpp, k_pscl], [k_mixo])
                    row = 16 + gi * 4 + jo
                    S.op("act", lambda e, row=row, mixo=mixo: e.dma_start(out=mixT_d[row * 128:(row + 1) * 128, :], in_=mixo),
                         reads=[k_mixo], writes=[("mx", row)], dma=True)

        def phase_B():
            phase_B_pools()
            S.barrier()
            phase_B_heads()

        P2 = pmat_end
        ones_f, k_onesf = A.carve("ones_f", P2, 512)
        n2gT, k_n2g = A.carve("n2gT", P2 + 512, 128)
        A2, k_A2 = A.carve("A2", P2 + 640, 128)
        ssq2, k_ssq2 = A.carve("ssq2", P2 + 768, 512)
        rstd2, k_rstd2 = A.carve("rstd2", P2 + 1280, 64)
        iota256, k_iota = A.carve("iota256", P2 + 1344, 1024)
        tokidx, k_tokidx = A.carve("tokidx", P2 + 2368, 64)
        idx_i, k_idx = A.carve("idx_i", P2 + 2432, 128, I32)
        gate, k_gate = A.carve("gate", P2 + 2560, 128)
        idx_f, k_idxf = A.carve("idx_f", P2 + 2688, 128)
        fss, k_fss = A.carve("fss", P2 + 2816, 64)
        assert P2 + 2880 <= SB_BYTES

        def bcast_row(col, kcol, dst, kdst):
            for g0 in range(0, 32, 4):
                bp, kbp = bank("bcps", (g0 // 4) % 2 + 6)
                for i in range(4):
                    kc = g0 + i
                    dg, kdg = A.carve("diag", P2 + 2880 + (kc % 2) * 512, 512)
                    OP("dve", lambda e, dg=dg, kc=kc: e.tensor_scalar(out=dg, in0=ident_f, scalar1=col[:, kc:kc + 1], scalar2=None, op0=ALU.mult),
                       [k_idf, kcol], [kdg])
                    OP("pe", lambda e, bp=bp, i=i, dg=dg: e.matmul(bp[:, i * 128:(i + 1) * 128], ones_f, dg, start=True, stop=True),
                       [k_onesf, kdg], [kbp])
                OP("act", lambda e, bp=bp, g0=g0: e.copy(out=dst[:, g0 * 128:(g0 + 4) * 128], in_=bp), [kbp], [kdst])

        def phase_C():
            K = 1024
            ld(n2gT, n2gT_d, k_n2g)
            OP("pool", lambda e: e.memset(ones_f, 1.0), [], [k_onesf])
            OP("pool", lambda e: e.memset(ssq2, 0.0), [], [k_ssq2])
            G1b, k_G1b = A.carve("G1b", 144 * K, 16384)
            g1col, k_g1c = A.carve("g1col", P2 + 2880 + 1024, 128)
            OP("dve", lambda e: e.tensor_copy(out=g1col, in_=mods3[:, 64:96, 0]), [k_mods], [k_g1c])
            bcast_row(g1col, k_g1c, G1b, k_G1b)
            for hf in range(2):
                mixh = []
                for kc in range(32):
                    mb, kmb = A.carve("mixh", kc * 2048, 2048, BF16)
                    S.op("sp", lambda e, mb=mb, kc=kc, hf=hf: e.dma_start(out=mb, in_=mixT_d[kc * 128:(kc + 1) * 128, hf * 1024:(hf + 1) * 1024]),
                         reads=[("mx", kc)], writes=[kmb], dma=True)
                    mixh.append((mb, kmb))
                for cb in range(8):
                    wbs = []
                    for pc in range(8):
                        st, kst = A.carve("wstC", 128 * K + (pc % 2) * 8192, 8192)
                        S.op("sp", lambda e, st=st, pc=pc, cb=cb: e.dma_start(
                            out=st.rearrange("p (k n) -> p k n", n=512),
                            in_=w_out_d[pc * 512:(pc + 1) * 512, cb * 512:(cb + 1) * 512].rearrange("(k p) n -> p k n", p=128)),
                            writes=[kst], dma=True)
                        wb, kwb = A.carve("wbC", 64 * K + (cb % 2) * 32768 + pc * 4096, 4096, BF16)
                        OP("pool", lambda e, wb=wb, st=st: e.tensor_copy(out=wb, in_=st), [kst], [kwb])
                        wbs.append((wb, kwb))
                    for tt in range(8):
                        ps_, kps = bank("cps", tt)
                        for kc in range(32):
                            wb, kwb = wbs[kc // 4]
                            mb, kmb = mixh[kc]
                            OP("pe", lambda e, ps_=ps_, mb=mb, wb=wb, kc=kc, tt=tt: e.matmul(
                                ps_, mb[:, tt * 128:(tt + 1) * 128], wb[:, (kc % 4) * 512:(kc % 4 + 1) * 512],
                                start=(kc == 0), stop=(kc == 31)), [kmb, kwb], [kps])
                        gt = hf * 8 + tt
                        xt, kxt = A.carve("xtC", 160 * K + (tt % 2) * 2048, 2048)
                        S.op("sp", lambda e, xt=xt, gt=gt, cb=cb: e.dma_start(out=xt, in_=x_d[gt * 128:(gt + 1) * 128, cb * 512:(cb + 1) * 512]),
                             writes=[kxt], dma=True)
                        xo, kxo = A.carve("xoC", 164 * K + (tt % 2) * 2048, 2048)
                        OP("dve", lambda e, xo=xo, ps_=ps_, cb=cb: e.tensor_tensor(out=xo, in0=ps_, in1=G1b[:, cb * 512:(cb + 1) * 512], op=ALU.mult),
                           [kps, k_G1b], [kxo])
                        OP("dve", lambda e, xo=xo, xt=xt: e.tensor_tensor(out=xo, in0=xo, in1=xt, op=ALU.add), [kxo, kxt], [kxo])
                        OP("act", lambda e, xt=xt, xo=xo, gt=gt, cb=cb: e.activation(
                            out=xt, in_=xo, func=AF.Square, accum_out=ssq2[:, gt * 8 + cb: gt * 8 + cb + 1]), [kxo, k_ssq2], [kxt, k_ssq2])
                        S.op("act", lambda e, xo=xo, gt=gt, cb=cb: e.dma_start(out=x1_d[gt * 128:(gt + 1) * 128, cb * 512:(cb + 1) * 512], in_=xo),
                             reads=[kxo], writes=[("x1", gt)], dma=True)

        afft_g = []

        def phase_D():
            K = 1024
            ld(iota256, iota_d, k_iota)
            ld(tokidx, tokidx_d, k_tokidx)
            OP("dve", lambda e: e.reduce_sum(out=rstd2, in_=ssq2.rearrange("p (t c) -> p t c", c=8), axis=AXX), [k_ssq2], [k_rstd2])
            OP("dve", lambda e: e.tensor_scalar(out=rstd2, in0=rstd2, scalar1=1.0 / D, scalar2=EPS, op0=ALU.mult, op1=ALU.add), [k_rstd2], [k_rstd2])
            OP("act", lambda e: e.activation(out=rstd2, in_=rstd2, func=AF.Sqrt), [k_rstd2], [k_rstd2])
            OP("dve", lambda e: e.reciprocal(out=rstd2, in_=rstd2), [k_rstd2], [k_rstd2])
            OP("dve", lambda e: e.scalar_tensor_tensor(out=A2, in0=mods3[:, 128:160, 0], scalar=1.0, in1=n2gT, op0=ALU.add, op1=ALU.mult),
               [k_mods, k_n2g], [k_A2])
            A2b, k_A2b = A.carve("A2b", 32 * K, 16384)
            B2b, k_B2b = A.carve("B2b", 48 * K, 16384)
            b2col, k_b2c = A.carve("b2col", P2 + 2880 + 1024, 128)
            OP("dve", lambda e: e.tensor_copy(out=b2col, in_=mods3[:, 96:128, 0]), [k_mods], [k_b2c])
            bcast_row(A2, k_A2, A2b, k_A2b)
            bcast_row(b2col, k_b2c, B2b, k_B2b)
            Rw, k_Rw = A.carve("Rw", 96 * K, 2048)
            ld(Rw, rwT_d, k_Rw)
            afft, k_afft = A.carve("afft", 98 * K, 1024)
            valT, k_valT = A.carve("valT", 99 * K, 1024)
            afft_g.extend([afft, k_afft, valT, k_valT])
            R2, k_R2 = A.carve("R2", 100 * K, 2048)
            affT, k_affT = A.carve("affT", 104 * K, 8192, parts=16)
            wk, k_wk = A.carve("wk", 112 * K, 8192, parts=16)
            msk, k_msk = A.carve("msk", 120 * K, 8192, parts=16)
            onesk, k_onesk = A.carve("onesk", 128 * K, 8192, parts=16)
            mx8, k_mx8 = A.carve("mx8", 136 * K, 32, parts=16)
            h2T, k_h2T = A.carve("h2T", 80 * K, 16384)
            sm, k_sm = A.carve("smx", 136 * K + 64, 64)
            for tt in range(16):
                x1t, kx1 = A.carve("x1t", (tt % 2) * 16384, 16384)
                S.op("sp", lambda e, x1t=x1t, tt=tt: e.dma_start(out=x1t, in_=x1_d[tt * 128:(tt + 1) * 128, :]),
                     reads=[("x1", tt)], writes=[kx1], dma=True)
                OP("dve", lambda e, x1t=x1t, tt=tt: e.scalar_tensor_tensor(out=x1t, in0=x1t, scalar=rstd2[:, tt:tt + 1], in1=A2b,
                                                                           op0=ALU.mult, op1=ALU.mult), [kx1, k_rstd2, k_A2b], [kx1])
                OP("pool", lambda e, x1t=x1t: e.tensor_tensor(out=x1t, in0=x1t, in1=B2b, op=ALU.add), [kx1, k_B2b], [kx1])
                h2b, kh2b = A.carve("h2b", 64 * K + (tt % 2) * 8192, 8192, BF16)
                OP("act", lambda e, h2b=h2b, x1t=x1t: e.copy(out=h2b, in_=x1t), [kx1], [kh2b])
                S.op("act", lambda e, h2b=h2b, tt=tt: e.dma_start(out=h2_d[tt * 128:(tt + 1) * 128, :], in_=h2b),
                     reads=[kh2b], writes=[("h2d", tt)], dma=True)
                for g0 in range(0, 32, 4):
                    tp, ktp = bank("tpD", (g0 // 4) % 2 + 4)
                    for i in range(4):
                        kc = g0 + i
                        OP("pe", lambda e, tp=tp, i=i, kc=kc, x1t=x1t: e.transpose(tp[:, i * 128:(i + 1) * 128], x1t[:, kc * 128:(kc + 1) * 128], ident_f),
                           [kx1, k_idf], [ktp])
                    eng = "act" if (g0 // 4) % 2 == 0 else "dve"
                    if eng == "act":
                        OP("act", lambda e, tp=tp, g0=g0: e.copy(out=h2T[:, g0 * 128:(g0 + 4) * 128], in_=tp), [ktp], [k_h2T])
                    else:
                        OP("dve", lambda e, tp=tp, g0=g0: e.tensor_copy(out=h2T[:, g0 * 128:(g0 + 4) * 128], in_=tp), [ktp], [k_h2T])
                lg, klg = bank("lgps", 6)
                for kc in range(32):
                    OP("pe", lambda e, lg=lg, kc=kc: e.matmul(lg[:, 0:16], h2T[:, kc * 128:(kc + 1) * 128], Rw[:, kc * 16:(kc + 1) * 16],
                                                              start=(kc == 0), stop=(kc == 31)), [k_h2T, k_Rw], [klg])
                OP("dve", lambda e, lg=lg: e.reduce_max(out=sm[:, 0:1], in_=lg[:, 0:16], axis=AXX), [klg], [k_sm])
                OP("dve", lambda e: e.tensor_scalar(out=sm[:, 1:2], in0=sm[:, 0:1], scalar1=-1.0, scalar2=None, op0=ALU.mult), [k_sm], [k_sm])
                OP("pool", lambda e: e.memset(sm[:, 2:3], 0.0), [], [k_sm])
                OP("act", lambda e, lg=lg, tt=tt: e.activation(out=afft[:, tt * 16:(tt + 1) * 16], in_=lg[:, 0:16], func=AF.Exp, bias=sm[:, 1:2],
                                                               scale=1.0, accum_out=sm[:, 2:3]), [klg, k_sm], [k_afft, k_sm])
                OP("dve", lambda e: e.reciprocal(out=sm[:, 3:4], in_=sm[:, 2:3]), [k_sm], [k_sm])
                OP("dve", lambda e, tt=tt: e.tensor_scalar(out=afft[:, tt * 16:(tt + 1) * 16], in0=afft[:, tt * 16:(tt + 1) * 16], scalar1=sm[:, 3:4],
                                                           scalar2=None, op0=ALU.mult), [k_afft, k_sm], [k_afft])
                t2, kt2 = bank("t2ps", 7)
                OP("pe", lambda e, t2=t2, tt=tt: e.transpose(t2[0:16, 0:128], afft[:, tt * 16:(tt + 1) * 16], ident_f), [k_afft, k_idf], [kt2])
                OP("act", lambda e, t2=t2, tt=tt: e.copy(out=affT[:, tt * 128:(tt + 1) * 128], in_=t2[0:16, 0:128]), [kt2], [k_affT])
            OP("dve", lambda e: e.tensor_copy(out=wk, in_=affT), [k_affT], [k_wk])
            OP("pool", lambda e: e.memset(onesk, 1.0), [], [k_onesk])
            for r in range(CAP // 8):
                OP("dve", lambda e: e.max(out=mx8, in_=wk), [k_wk], [k_mx8])
                if r < CAP // 8 - 1:
                    OP("dve", lambda e: e.match_replace(out=wk, in_to_replace=mx8, in_values=wk, imm_value=-1.0), [k_wk, k_mx8], [k_wk])
            OP("dve", lambda e: e.tensor_scalar(out=msk, in0=affT, scalar1=mx8[:, 7:8], scalar2=None, op0=ALU.is_ge), [k_affT, k_mx8], [k_msk])
            OP("dve", lambda e: e.tensor_tensor_scan(out=wk, data0=onesk, data1=msk, initial=0.0, op0=ALU.mult, op1=ALU.add), [k_onesk, k_msk], [k_wk])
            OP("dve", lambda e: e.tensor_tensor(out=wk, in0=wk, in1=msk, op=ALU.mult), [k_wk, k_msk], [k_wk])
            for tt in range(16):
                t2, kt2 = bank("t2ps", 7)
                OP("pe", lambda e, t2=t2, tt=tt: e.transpose(t2[:, 0:16], wk[:, tt * 128:(tt + 1) * 128], ident_f[0:16, 0:16]), [k_wk, k_idf], [kt2])
                OP("act", lambda e, t2=t2, tt=tt: e.copy(out=valT[:, tt * 16:(tt + 1) * 16], in_=t2[:, 0:16]), [kt2], [k_valT])
            R2v = R2.rearrange("p (t e two) -> p t e two", e=16, two=2)
            OP("dve", lambda e: e.tensor_copy(out=R2v[:, :, :, 1], in_=afft.rearrange("p (t e) -> p t e", e=16)), [k_afft], [k_R2])
            OP("dve", lambda e: e.tensor_copy(out=R2v[:, :, :, 0], in_=tokidx.unsqueeze(2).to_broadcast([128, 16, 16])), [k_tokidx], [k_R2])
            ig0, kig0 = bank("ig0", 4)
            ig1, kig1 = bank("ig1", 5)
            igs = ((ig0, kig0), (ig1, kig1))
            for ex in range(NE):
                for tt in range(16):
                    sel, ksel = A.carve("sel", 102 * K + (tt % 2) * 1024, 1024)
                    OP("dve", lambda e, sel=sel, tt=tt, ex=ex: e.tensor_scalar(out=sel, in0=iota256, scalar1=valT[:, tt * 16 + ex: tt * 16 + ex + 1],
                                                                               scalar2=None, op0=ALU.is_equal), [k_iota, k_valT], [ksel])
                    for jh in range(2):
                        ig, kig = igs[jh]
                        OP("pe", lambda e, ig=ig, sel=sel, jh=jh, tt=tt, ex=ex: e.matmul(
                            ig[:, ex * 2:ex * 2 + 2], sel[:, jh * 128:(jh + 1) * 128], R2[:, (tt * 16 + ex) * 2:(tt * 16 + ex) * 2 + 2],
                            start=(tt == 0), stop=(tt == 15)), [ksel, k_R2], [kig])
            for jh in range(2):
                ig, kig = igs[jh]
                igv = ig[:, 0:32].rearrange("p (e two) -> p e two", two=2)
                OP("dve", lambda e, igv=igv, jh=jh: e.tensor_copy(out=idx_f.rearrange("p (e j) -> p e j", j=2)[:, :, jh], in_=igv[:, :, 0]), [kig], [k_idxf])
                OP("dve", lambda e, igv=igv, jh=jh: e.tensor_copy(out=gate.rearrange("p (e j) -> p e j", j=2)[:, :, jh], in_=igv[:, :, 1]), [kig], [k_gate])
            OP("dve", lambda e: e.tensor_copy(out=idx_i, in_=idx_f), [k_idxf], [k_idx])

        def phase_E():
            K = 1024
            import concourse.bass as _b
            h2keys = [("h2d", tt) for tt in range(16)]
            zt, kzt = A.carve("zt", 146 * K, 8192)
            OP("pool", lambda e: e.memset(zt, 0.0), [], [kzt])
            for dh in range(2):
                for tt in range(16):
                    S.op("sp", lambda e, dh=dh, tt=tt: e.dma_start(out=moe_d[dh][tt * 128:(tt + 1) * 128, :], in_=zt),
                         reads=[kzt], writes=[("moe", dh)], dma=True)
            ncast = [0]

            def cast(dst, src, ksrc, kdst):
                eng = ("pool", "dve", "act")[ncast[0] % 3]
                ncast[0] += 1
                if eng == "act":
                    OP("act", lambda e: e.copy(out=dst, in_=src), [ksrc], [kdst])
                else:
                    OP(eng, lambda e: e.tensor_copy(out=dst, in_=src), [ksrc], [kdst])

            for ex in range(NE):
                S.barrier()
                xgT, k_xgT = A.carve("xgT", 16 * K, 16384, BF16)
                for jh in range(2):
                    col = ex * 2 + jh
                    xg, kxg = A.carve("xg", jh * 8192, 8192, BF16)
                    S.op("pool", lambda e, xg=xg, col=col: e.indirect_dma_start(
                        out=xg, out_offset=None, in_=h2_d[:, :], in_offset=_b.IndirectOffsetOnAxis(ap=idx_i[:, col:col + 1], axis=0)), reads=h2keys + [k_idx], writes=[kxg], dma=True)
                    for g0 in range(0, 32, 4):
                        tp, ktp = bank("tpE", (g0 // 4) % 2 + 6, 1, BF16)
                        for i in range(4):
                            kc = g0 + i
                            OP("pe", lambda e, tp=tp, i=i, kc=kc, xg=xg: e.transpose(tp[:, i * 128:(i + 1) * 128], xg[:, kc * 128:(kc + 1) * 128], ident_b),
                               [kxg, k_idb], [ktp])
                        dst = xgT.rearrange("p (k s) -> p k s", s=256)[:, g0:g0 + 4, jh * 128:(jh + 1) * 128]
                        OP("act", lambda e, tp=tp, dst=dst: e.copy(out=dst, in_=tp[:, 0:512].rearrange("p (k s) -> p k s", s=128)), [ktp], [k_xgT])
                hidT, k_hid = A.carve("hidT", 88 * K, 8192, BF16)
                for fc in range(16):
                    hps = []
                    for wi, wd in enumerate((w1_d, w3_d)):
                        st, kst = A.carve("wstE", 32 * K + ((fc * 2 + wi) % 2) * 16384, 16384)
                        wb, kwb = A.carve("wbE", 64 * K + ((fc * 2 + wi) % 3) * 8192, 8192, BF16)
                        for q4 in range(4):
                            stq, kstq = A.carve("wstEq", 32 * K + ((fc * 2 + wi) % 2) * 16384 + q4 * 4096, 4096)
                            S.op("sp", lambda e, stq=stq, wd=wd, q4=q4, fc=fc, ex=ex: e.dma_start(
                                out=stq.rearrange("p (k n) -> p k n", n=128),
                                in_=wd[ex, q4 * 1024:(q4 + 1) * 1024, fc * 128:(fc + 1) * 128].rearrange("(k p) n -> p k n", p=128)),
                                writes=[kstq], dma=True)
                            wbq, kwbq = A.carve("wbEq", 64 * K + ((fc * 2 + wi) % 3) * 8192 + q4 * 2048, 2048, BF16)
                            cast(wbq, stq, kstq, kwbq)
                            if q4 == 0:
                                qs = []
                            qs.append((wbq, kwbq))
                        hp, khp = bank("hps", (fc % 2) * 2 + wi)
                        for kc in range(32):
                            wbq, kwbq = qs[kc // 8]
                            OP("pe", lambda e, hp=hp, wbq=wbq, kc=kc: e.matmul(
                                hp[:, 0:256], wbq[:, (kc % 8) * 128:(kc % 8 + 1) * 128], xgT[:, kc * 256:(kc + 1) * 256],
                                start=(kc == 0), stop=(kc == 31)), [kwbq, k_xgT], [khp])
                        hps.append((hp, khp))
                    (h1, kh1), (h3, kh3) = hps
                    tm, ktm = A.carve("tmE", 96 * K + (fc % 2) * 1024, 1024)
                    OP("act", lambda e, tm=tm, h1=h1: e.activation(out=tm, in_=h1[:, 0:256], func=AF.Exp, scale=-1.0), [kh1], [ktm])
                    OP("dve", lambda e, tm=tm: e.tensor_scalar(out=tm, in0=tm, scalar1=1.0, scalar2=None, op0=ALU.add), [ktm], [ktm])
                    OP("dve", lambda e, tm=tm: e.reciprocal(out=tm, in_=tm), [ktm], [ktm])
                    OP("dve", lambda e, tm=tm, h1=h1: e.tensor_tensor(out=tm, in0=tm, in1=h1[:, 0:256], op=ALU.mult), [ktm, kh1], [ktm])
                    OP("dve", lambda e, tm=tm, h3=h3, fc=fc: e.tensor_tensor(out=hidT[:, fc * 256:(fc + 1) * 256], in0=tm, in1=h3[:, 0:256], op=ALU.mult),
                       [ktm, kh3], [k_hid])
                ybs = [A.carve("ybuf", 146 * K + jh * 8192, 8192) for jh in range(2)]
                for db in range(8):
                    w2q = []
                    for pc in range(2):
                        st, kst = A.carve("wst2", 98 * K + ((db * 2 + pc) % 2) * 16384, 16384)
                        S.op("sp", lambda e, st=st, pc=pc, db=db, ex=ex: e.dma_start(
                            out=st.rearrange("p (k n) -> p k n", n=512),
                            in_=w2_d[ex, pc * 1024:(pc + 1) * 1024, db * 512:(db + 1) * 512].rearrange("(k p) n -> p k n", p=128)),
                            writes=[kst], dma=True)
                        wb, kwb = A.carve("wb2", 130 * K + ((db * 2 + pc) % 2) * 8192, 8192, BF16)
                        cast(wb, st, kst, kwb)
                        w2q.append((wb, kwb))
                    for jh in range(2):
                        yp, kyp = bank("yps", 4 + jh)
                        for k16 in range(16):
                            wb, kwb = w2q[k16 // 8]
                            OP("pe", lambda e, yp=yp, wb=wb, k16=k16, jh=jh: e.matmul(
                                yp, hidT[:, k16 * 256 + jh * 128: k16 * 256 + (jh + 1) * 128], wb[:, (k16 % 8) * 512:(k16 % 8 + 1) * 512],
                                start=(k16 == 0), stop=(k16 == 15)), [k_hid, kwb], [kyp])
                        yb, kyb = ybs[jh]
                        col = ex * 2 + jh
                        OP("act", lambda e, yb=yb, yp=yp, db=db, col=col: e.activation(
                            out=yb[:, (db % 4) * 512:(db % 4 + 1) * 512], in_=yp, func=AF.Identity, scale=gate[:, col:col + 1]), [kyp, k_gate], [kyb])
                        if db % 4 == 3:
                            dh = db // 4
                            S.op("pool", lambda e, yb=yb, col=col, dh=dh: e.indirect_dma_start(
                                out=moe_d[dh][:, :], out_offset=_b.IndirectOffsetOnAxis(ap=idx_i[:, col:col + 1], axis=0),
                                in_=yb, in_offset=None, compute_op=ALU.add),
                                reads=[kyb, k_idx], writes=[("moe", dh)], dma=True)

        def phase_F():
            K = 1024
            G2b, k_G2b = A.carve("G2b", 96 * K, 16384)
            FNb, k_FNb = A.carve("FNb", 112 * K, 16384)
            g2col, k_g2c = A.carve("g2col", P2 + 2880 + 1024, 128)
            fncol, k_fnc = A.carve("fncol", P2 + 2880 + 1152, 128)
            OP("dve", lambda e: e.tensor_copy(out=g2col, in_=mods3[:, 160:192, 0]), [k_mods], [k_g2c])
            ld(fncol, fngT_d, k_fnc)
            bcast_row(g2col, k_g2c, G2b, k_G2b)
            bcast_row(fncol, k_fnc, FNb, k_FNb)
            for tt in range(16):
                x1t, kx1 = A.carve("x1tF", (tt % 2) * 16384, 16384)
                mo, kmo = A.carve("moF", 32 * K + (tt % 2) * 16384, 16384)
                jk, kjk = A.carve("jkF", 64 * K, 16384)
                S.op("sp", lambda e, x1t=x1t, tt=tt: e.dma_start(out=x1t, in_=x1_d[tt * 128:(tt + 1) * 128, :]),
                     reads=[("x1", tt)], writes=[kx1], dma=True)
                for dh in range(2):
                    S.op("sp", lambda e, mo=mo, tt=tt, dh=dh: e.dma_start(out=mo[:, dh * 2048:(dh + 1) * 2048], in_=moe_d[dh][tt * 128:(tt + 1) * 128, :]),
                         reads=[("moe", dh)], writes=[kmo], dma=True)
                OP("dve", lambda e, mo=mo: e.tensor_tensor(out=mo, in0=mo, in1=G2b, op=ALU.mult), [kmo, k_G2b], [kmo])
                OP("pool", lambda e, mo=mo, x1t=x1t: e.tensor_tensor(out=mo, in0=mo, in1=x1t, op=ALU.add), [kmo, kx1], [kmo])
                OP("pool", lambda e: e.memset(fss[:, 0:1], 0.0), [], [k_fss])
                OP("act", lambda e, jk=jk, mo=mo: e.activation(out=jk, in_=mo, func=AF.Square, accum_out=fss[:, 0:1]), [kmo, k_fss], [kjk, k_fss])
                OP("dve", lambda e: e.tensor_scalar(out=fss[:, 1:2], in0=fss[:, 0:1], scalar1=1.0 / D, scalar2=EPS, op0=ALU.mult, op1=ALU.add), [k_fss], [k_fss])
                OP("act", lambda e: e.activation(out=fss[:, 2:3], in_=fss[:, 1:2], func=AF.Sqrt), [k_fss], [k_fss])
                OP("dve", lambda e: e.reciprocal(out=fss[:, 3:4], in_=fss[:, 2:3]), [k_fss], [k_fss])
                OP("dve", lambda e, mo=mo, x1t=x1t: e.scalar_tensor_tensor(out=x1t, in0=mo, scalar=fss[:, 3:4], in1=FNb, op0=ALU.mult, op1=ALU.mult),
                   [kmo, k_fss, k_FNb], [kx1])
                S.op("act", lambda e, x1t=x1t, tt=tt: e.dma_start(out=out_d[tt * 128:(tt + 1) * 128, :], in_=x1t),
                     reads=[kx1], dma=True, is_out=True)

        if upto in ("p1", "A"):
            pass
        else:
            S.barrier()
            init_B_consts()
            S.barrier()
            try:
                phase_B()
            except _Stop:
                pass
            S.barrier()
            if upto != "B":
                phase_C()
                S.barrier()
                if upto != "C":
                    phase_D()
                    S.barrier()
                    if upto != "D":
                        phase_E()
                        S.barrier()
                        phase_F()

        if dbg and upto in ("C", "D"):
            dbg_out["x1"] = dout("d_x1", [L, D])
            for i in range(16):
                t, kt = A.carve("cp", (i % 2) * 16384, 16384)
                S.op("sp", lambda e, t=t, i=i: e.dma_start(out=t, in_=x1_d[i * 128:(i + 1) * 128, :]),
                     reads=[("x1", i)], writes=[kt], dma=True)
                S.op("sp", lambda e, t=t, i=i: e.dma_start(out=dbg_out["x1"][i * 128:(i + 1) * 128, :], in_=t),
                     reads=[kt], dma=True, is_out=True)
        if dbg and upto in ("D", "all"):
            dbg_out["afft"] = dout("d_afft", [128, 256])
            dbg_out["gate"] = dout("d_gate", [128, 32])
            dbg_out["idxf"] = dout("d_idxf", [128, 32])
            dbg_out["valT"] = dout("d_valT", [128, 256])
            S.op("sp", lambda e: e.dma_start(out=dbg_out["afft"], in_=afft_g[0]), reads=[afft_g[1]], dma=True, is_out=True)
            S.op("sp", lambda e: e.dma_start(out=dbg_out["valT"], in_=afft_g[2]), reads=[afft_g[3]], dma=True, is_out=True)
            S.op("sp", lambda e: e.dma_start(out=dbg_out["gate"], in_=gate), reads=[k_gate], dma=True, is_out=True)
            S.op("sp", lambda e: e.dma_start(out=dbg_out["idxf"], in_=idx_f), reads=[k_idxf], dma=True, is_out=True)
        if dbg and upto == "B":
            dbg_out["mixT"] = dout("d_mixT", [D, L], BF16)
            for i in range(32):
                t, kt = A.carve("cp", (i % 2) * 4096, 4096, BF16)
                S.op("sp", lambda e, t=t, i=i: e.dma_start(out=t, in_=mixT_d[i * 128:(i + 1) * 128, :]),
                     reads=[("mx", i)], writes=[kt], dma=True)
                S.op("sp", lambda e, t=t, i=i: e.dma_start(out=dbg_out["mixT"][i * 128:(i + 1) * 128, :], in_=t),
                     reads=[kt], dma=True, is_out=True)
        if dbg and upto == "A":
            dbg_out["projT"] = dout("d_projT", [INC, L])
            dbg_out["cprojT"] = dout("d_cprojT", [3 * HGW, CTX])
            for i in range(96):
                t, kt = A.carve("cp", 0, 8192)
                S.op("sp", lambda e, t=t, i=i: e.dma_start(out=t, in_=projT_d[i * 128:(i + 1) * 128, :]),
                     reads=[("pj", i)], writes=[kt], dma=True)
                S.op("sp", lambda e, t=t, i=i: e.dma_start(out=dbg_out["projT"][i * 128:(i + 1) * 128, :], in_=t),
                     reads=[kt], dma=True, is_out=True)
            for i in range(48):
                t, kt = A.carve("cp", 0, 1024)
                S.op("sp", lambda e, t=t, i=i: e.dma_start(out=t, in_=cprojT_d[i * 128:(i + 1) * 128, :]),
                     reads=[("cpj", i)], writes=[kt], dma=True)
                S.op("sp", lambda e, t=t, i=i: e.dma_start(out=dbg_out["cprojT"][i * 128:(i + 1) * 128, :], in_=t),
                     reads=[kt], dma=True, is_out=True)

        S.finish()
        S.emit(nc)
    return nc


def make_inputs(inp, b):
    f = lambda a: np.ascontiguousarray(a, dtype=np.float32)
    c2 = np.stack([inp["c"][b], inp["c_ctx"]], axis=1)
    c2T = c2.reshape(32, 128, 2).transpose(1, 0, 2).reshape(128, 64)
    colT = lambda v: np.ascontiguousarray(np.asarray(v).reshape(-1, 128).T)
    return {
        "x": f(inp["x"][b]),
        "ctx": f(inp["ctx"][b]),
        "c2T": f(c2T),
        "ada_w": f(inp["ada_w"][0]),
        "ada_bT": f(colT(inp["ada_b"][0])),
        "n1gT": f(colT(inp["norm1_g"][0])),
        "w_in": f(inp["w_in"][0]),
        "ident": np.eye(128, dtype=np.float32),
        "lbp": f(inp["lb_param"].reshape(2, 2, 16, 128).transpose(3, 0, 1, 2).reshape(128, 64)),
        "hgnT": f(colT(inp["hg_norm_g"][0])),
        "psclT": f(colT(inp["pool_scale"][0])),
        "mk": _consts()["mk"], "pmat": _consts()["pmat"], "icnt": _consts()["icnt"],
        "pool_w": f(inp["pool_w"][0]),
        "w_out": f(inp["w_out"][0]),
        "n2gT": f(colT(inp["norm2_g"][0])),
        "fngT": f(colT(inp["final_norm_g"])),
        "iota256": np.ascontiguousarray(np.broadcast_to(np.arange(1, 257, dtype=np.float32)[None, :], (128, 256))),
        "tokidx": np.ascontiguousarray((np.arange(16)[None, :] * 128 + np.arange(128)[:, None]).astype(np.float32)),
        "rwT": f(inp["router_w"][0].reshape(32, 128, 16).transpose(1, 0, 2).reshape(128, 512)),
        "moe_w1": f(inp["moe_w1"][0]), "moe_w3": f(inp["moe_w3"][0]), "moe_w2": f(inp["moe_w2"][0]),
    }


_CONST_CACHE = {}


def _consts():
    if _CONST_CACHE:
        return _CONST_CACHE
    p = np.arange(128)
    t = np.arange(64)
    mk = np.zeros((128, 128), np.float32)
    mk[:, 0:64] = ((p[:, None] % 64) <= t[None, :])
    mk[:, 64:128] = ((p[:, None] % 64) >= t[None, :])
    wins = (2, 4, 8, 16)
    offs = {0: (-1, 0), 1: (-1, 0, 1), 2: (-2, -1, 0, 1, 2), 3: (-4, -3, -2, -1, 0, 1, 2, 3, 4)}
    mats = []
    T = 8
    tl = np.arange(128)
    rt, ct = 2 * T + tl // 64, tl % 64
    for gi, w in enumerate(wins):
        for o in offs[gi]:
            rs, cs = 2 * (T + o) + tl // 64, tl % 64
            m = ((rs[:, None] >= rt[None, :] - w // 2) & (rs[:, None] <= rt[None, :] - w // 2 + w - 1) &
                 (cs[:, None] >= ct[None, :] - w // 2) & (cs[:, None] <= ct[None, :] - w // 2 + w - 1))
            mats.append(m.astype(np.float32))
    pmat = np.concatenate(mats, axis=1)
    icnt = np.zeros((4, 128, 2048), np.float32)
    for gi, w in enumerate(wins):
        def cntv(n):
            idx = np.arange(n)
            st = idx - w // 2
            return np.clip(st + w, 0, n) - np.clip(st, 0, n)
        cnt = (cntv(32)[:, None] * cntv(64)[None, :]).reshape(-1).astype(np.float32)
        icnt[gi] = (1.0 / cnt)[None, :]
    _CONST_CACHE.update(mk=mk, pmat=np.ascontiguousarray(pmat), icnt=icnt)
    return _CONST_CACHE


def kernel(**inputs):
    inp = {k: np.asarray(v) for k, v in inputs.items()}
    nc = build_program(upto="all", dbg=False)
    in_maps = [make_inputs(inp, b) for b in range(N_CORES)]
    res = run_bass_kernel_spmd(nc, in_maps, core_ids=list(range(N_CORES)))
    out = np.stack([np.asarray(r["out"]) for r in res.results], axis=0)
    return out.astype(np.float32)
```

```python
import numpy as np
from contextlib import ExitStack
import concourse.bass as bass
import concourse.mybir as mybir
from concourse.bass_utils import run_bass_kernel_spmd

F32 = mybir.dt.float32
BF16 = mybir.dt.bfloat16
I32 = mybir.dt.int32
AF = mybir.ActivationFunctionType
ALU = mybir.AluOpType

D = 4096
L = 2048
CTX = 256
HGW = 2048
NH = 16
INC = 12288
NE = 16
FF = 2048
CAP = 256
EPS = 1e-6
N_CORES = 4

ENGS = ("pe", "act", "dve", "pool", "sp")
N_DMA_SEMS = 12
HEAD_BARRIER = True


class Sched:
    def __init__(self):
        self.ops = {e: [] for e in ENGS}
        self.seq = {e: 0 for e in ENGS}
        self.seen = {e: {} for e in ENGS}
        self.last_w = {}
        self.readers = {}
        self.dma_cnt = [0] * N_DMA_SEMS
        self.dma_rr = 0
        self.out_deps = []

    def _need(self, eng, dep, waits, war=False):
        if dep is None:
            return
        sk, val = dep
        if sk == eng and (eng == "pe" or war):
            return
        if self.seen[eng].get(sk, 0) >= val:
            return
        self.seen[eng][sk] = val
        waits.append((sk, val))

    def alias(self, newkey, oldkeys):
        rs = self.readers.setdefault(newkey, [])
        for k in oldkeys:
            if k in self.last_w and self.last_w[k] is not None:
                rs.append(self.last_w[k])
            rs.extend(self.readers.get(k, ()))

    def op(self, eng, fn, reads=(), writes=(), dma=False, is_out=False):
        waits = []
        for r in reads:
            self._need(eng, self.last_w.get(r), waits)
        for w in writes:
            self._need(eng, self.last_w.get(w), waits)
            for d in self.readers.get(w, ()):
                self._need(eng, d, waits, war=True)
        if dma:
            j = self.dma_rr
            self.dma_rr = (self.dma_rr + 1) % N_DMA_SEMS
            sk = ("dma", j)
            prev = self.dma_cnt[j]
            if prev > 0 and self.seen[eng].get(sk, 0) < prev:
                self.seen[eng][sk] = prev
                waits.append((sk, prev))
            self.dma_cnt[j] = prev + 16
            dep = (sk, prev + 16)
            inc = (sk, 16)
        else:
            self.seq[eng] += 1
            dep = (eng, self.seq[eng])
            inc = (eng, 1)
        self.ops[eng].append((waits, fn, inc))
        for w in writes:
            self.last_w[w] = dep
            self.readers[w] = []
        for r in reads:
            self.readers.setdefault(r, []).append(dep)
        if is_out:
            self.out_deps.append(dep)
        return dep

    def barrier(self):
        for eng in ENGS:
            waits = []
            for e2 in ENGS:
                if e2 != eng and self.seq[e2] > 0:
                    self._need(eng, (e2, self.seq[e2]), waits)
            if eng not in ("pe", "sp") and self.seq[eng] > 0:
                self._need(eng, (eng, self.seq[eng]), waits)
            for j in range(N_DMA_SEMS):
                if self.dma_cnt[j] > 0:
                    self._need(eng, (("dma", j), self.dma_cnt[j]), waits)
            if waits:
                self.ops[eng].append((waits, None, None))

    def finish(self):
        waits = []
        for d in self.out_deps:
            self._need("sp", d, waits)
        if waits:
            self.ops["sp"].append((waits, None, None))

    def emit(self, nc):
        with ExitStack() as es:
            sems = {}
            for e in ENGS:
                sems[e] = es.enter_context(nc.semaphore("s_" + e))
            for j in range(N_DMA_SEMS):
                sems[("dma", j)] = es.enter_context(nc.semaphore("s_dma%d" % j))
            block = es.enter_context(nc.Block())

            def run(engh, ename):
                for waits, fn, inc in self.ops[ename]:
                    for sk, val in waits:
                        engh.wait_ge(sems[sk], val)
                    if fn is None:
                        continue
                    try:
                        ins = fn(engh)
                    except Exception:
                        print("EMIT FAIL", ename, "op#", self.ops[ename].index((waits, fn, inc)), "of", len(self.ops[ename]))
                        raise
                    ins.then_inc(sems[inc[0]], inc[1])

            block.tensor(lambda e: run(e, "pe"))
            block.scalar(lambda e: run(e, "act"))
            block.vector(lambda e: run(e, "dve"))
            block.gpsimd(lambda e: run(e, "pool"))
            block.sync(lambda e: run(e, "sp"))


class Arena:
    def __init__(self, S, big, nbytes):
        self.S = S
        self.big = big
        self.nbytes = nbytes
        self.live = []
        self.cnt = 0

    def carve(self, name, lo, nbytes, dt=F32, parts=128):
        hi = lo + nbytes
        assert hi <= self.nbytes and lo % 4 == 0 and nbytes % 4 == 0, (name, lo, nbytes)
        self.cnt += 1
        key = "%s#%d" % (name, self.cnt)
        keep, old = [], []
        for (a, b, k) in self.live:
            if a < hi and lo < b:
                old.append(k)
                if a < lo:
                    keep.append((a, lo, k))
                if hi < b:
                    keep.append((hi, b, k))
            else:
                keep.append((a, b, k))
        self.S.alias(key, old)
        keep.append((lo, hi, key))
        self.live = keep
        ap = self.big[0:parts, lo // 4:hi // 4]
        if dt != F32:
            ap = ap.bitcast(dt)
        return ap, key


class _Stop(Exception):
    pass


def build_program(upto="all", dbg=False, sa=None, nhb=NH, npool=4, bstop=99):
    nc = bass.Bass("TRN2", target_bir_lowering=False)
    S = Sched()

    SA_KEEP = {"B": {"ident", "lbp", "hgnT", "psclT", "mk", "pmat", "icnt", "pool_w", "projT", "cprojT"}}

    def din(name, shape, dt=F32):
        if sa is not None and name not in SA_KEEP[sa]:
            return nc.dram_tensor(name, [128, 128], dt, kind="Internal").ap()
        return nc.dram_tensor(name, list(shape), dt, kind="ExternalInput").ap()

    def dscr(name, shape, dt=F32):
        return nc.dram_tensor(name, list(shape), dt, kind="Internal").ap()

    def dout(name, shape, dt=F32):
        return nc.dram_tensor(name, list(shape), dt, kind="ExternalOutput").ap()

    x_d = din("x", [L, D])
    ctx_d = din("ctx", [CTX, D])
    c2T_d = din("c2T", [128, 64])
    ada_w_d = din("ada_w", [D, 6 * D])
    ada_bT_d = din("ada_bT", [128, 192])
    n1gT_d = din("n1gT", [128, 32])
    w_in_d = din("w_in", [D, INC])
    ident_d = din("ident", [128, 128])
    lbp_d = din("lbp", [128, 64])
    hgnT_d = din("hgnT", [128, 16])
    psclT_d = din("psclT", [128, 16])
    mk_d = din("mk", [128, 128])
    pmat_d = din("pmat", [128, 19 * 128])
    icnt_d = din("icnt", [4, 128, 2048])
    pool_w_d = din("pool_w", [4, 512, 512])
    mixT_d = dscr("mixT", [D, L], BF16)
    w_out_d = din("w_out", [D, D])
    n2gT_d = din("n2gT", [128, 32])
    fngT_d = din("fngT", [128, 32])
    iota_d = din("iota256", [128, 256])
    tokidx_d = din("tokidx", [128, 16])
    rwT_d = din("rwT", [128, 512])
    w1_d = din("moe_w1", [NE, D, FF])
    w3_d = din("moe_w3", [NE, D, FF])
    w2_d = din("moe_w2", [NE, FF, D])
    x1_d = dscr("x1", [L, D])
    h2_d = dscr("h2", [L, D], BF16)
    moe_d = [dscr("moe0", [L, 2048]), dscr("moe1", [L, 2048])]
    out_d = dout("out", [L, D])
    projT_d = (din if sa == "B" else dscr)("projT", [INC, L])
    cprojT_d = (din if sa == "B" else dscr)("cprojT", [3 * HGW, CTX])
    dbg_out = {}
    if dbg:
        dbg_out["mods"] = dout("d_mods", [128, 384])
        dbg_out["hT"] = dout("d_hT", [128, 32 * 1024], BF16)

    es = ExitStack()
    with es, nc.allow_low_precision("bf16 matmul operands, fp32 accumulation"):
        SB_BYTES = 206 * 1024
        big = es.enter_context(nc.sbuf_tensor("arena", [128, SB_BYTES // 4], F32))
        psum = es.enter_context(nc.psum_tensor("psum", [128, 8 * 512], F32))
        A = Arena(S, big[:], SB_BYTES)
        PS = Arena(S, psum[:], 16 * 1024)

        def bank(name, b, nb=1, dt=F32):
            return PS.carve(name, b * 2048, nb * 2048, dt)

        P0 = 168 * 1024
        ident_f, k_idf = A.carve("ident_f", P0, 512)
        ident_b, k_idb = A.carve("ident_b", P0 + 512, 256, BF16)
        mods, k_mods = A.carve("mods", P0 + 768, 192 * 2 * 4)
        n1gT, k_n1g = A.carve("n1gT", P0 + 768 + 1536, 128)
        A1, k_A1 = A.carve("A1", P0 + 2432, 128)
        A1c, k_A1c = A.carve("A1c", P0 + 2560, 128)
        c2T, k_c2T = A.carve("c2T", P0 + 2688, 256)
        scT, k_scT = A.carve("scT", P0 + 2944, 256)
        adab, k_adab = A.carve("adab", P0 + 3200, 768)
        small, k_small = A.carve("small", P0 + 3968, 256)

        mods3 = mods.rearrange("p (j t) -> p j t", t=2)

        def ld(out_ap, in_ap, key, eng="sp"):
            S.op(eng, lambda e: e.dma_start(out=out_ap, in_=in_ap), writes=[key], dma=True)

        ld(ident_f, ident_d, k_idf)
        if sa is None:
            ld(c2T, c2T_d, k_c2T)
            ld(adab, ada_bT_d, k_adab)
            ld(n1gT, n1gT_d, k_n1g)
        S.op("dve", lambda e: e.tensor_copy(out=ident_b, in_=ident_f), reads=[k_idf], writes=[k_idb])
        if sa is None:
            S.op("act", lambda e: e.activation(out=scT, in_=c2T, func=AF.Silu), reads=[k_c2T], writes=[k_scT])

        if sa is None:
            mod_ps, k_modps = bank("mod_ps", 0)
            NB0 = 3
            wst0 = [A.carve("wst0_%d" % i, i * 16384, 16384) for i in range(NB0)]
            n_ld = 0
            PIECE = 4096
            for pc in range(6):
                for kc in range(32):
                    buf, bkey = wst0[n_ld % NB0]
                    n_ld += 1
                    src = ada_w_d[kc * 128:(kc + 1) * 128, pc * PIECE:(pc + 1) * PIECE]
                    ld(buf, src, bkey)
                    for j in range(32):
                        jj = pc * 32 + j
                        S.op("pe", (lambda e, buf=buf, j=j, jj=jj, kc=kc: e.matmul(
                            mod_ps[:, jj * 2:jj * 2 + 2], buf[:, j * 128:(j + 1) * 128], scT[:, kc * 2:kc * 2 + 2],
                            start=(kc == 0 and jj == 0), stop=(kc == 31), skip_group_check=True)),
                            reads=[bkey, k_scT], writes=[k_modps])
            S.op("dve", lambda e: e.tensor_tensor(out=mods3, in0=mod_ps[:, 0:384].rearrange("p (j t) -> p j t", t=2),
                                                  in1=adab.unsqueeze(2).to_broadcast([128, 192, 2]), op=ALU.add),
                 reads=[k_modps, k_adab], writes=[k_mods])
            S.op("dve", lambda e: e.scalar_tensor_tensor(out=A1, in0=mods3[:, 32:64, 0], scalar=1.0, in1=n1gT,
                                                         op0=ALU.add, op1=ALU.mult), reads=[k_mods, k_n1g], writes=[k_A1])
            S.op("dve", lambda e: e.scalar_tensor_tensor(out=A1c, in0=mods3[:, 32:64, 1], scalar=1.0, in1=n1gT,
                                                         op0=ALU.add, op1=ALU.mult), reads=[k_mods, k_n1g], writes=[k_A1c])
            if dbg:
                S.op("sp", lambda e: e.dma_start(out=dbg_out["mods"], in_=mods), reads=[k_mods], dma=True, is_out=True)

            HT_B = 64 * 1024
            hT, k_hT = None, None

            def norm_transpose(src_d, ntt, hdst, kdst, Acol, Bsel, t_off):
                for g0 in range(0, ntt, 4):
                    gn = min(4, ntt - g0)
                    xs_list = []
                    for t in range(gn):
                        xt, kx = A.carve("xt", HT_B + 16384 + (t % 2) * 16384, 16384)
                        ld(xt, src_d[(g0 + t) * 128:(g0 + t + 1) * 128, :], kx)
                        junk, kj = A.carve("junk", HT_B + 49152, 8192, BF16)
                        ss, kss = A.carve("ss", P0 + 4224 + 48 * t, 48)
                        S.op("pool", lambda e, ss=ss: e.memset(ss, 0.0), writes=[kss])
                        for i8 in range(8):
                            S.op("act", lambda e, xt=xt, junk=junk, ss=ss, i8=i8: e.activation(
                                out=junk[:, i8 * 512:(i8 + 1) * 512], in_=xt[:, i8 * 512:(i8 + 1) * 512], func=AF.Square,
                                accum_out=ss[:, 2 + i8:3 + i8]), reads=[kx, kss], writes=[kj, kss])
                        S.op("dve", lambda e, ss=ss: e.reduce_sum(out=ss[:, 0:1], in_=ss[:, 2:10], axis=mybir.AxisListType.X),
                             reads=[kss], writes=[kss])
                        S.op("dve", lambda e, ss=ss: e.tensor_scalar(out=ss[:, 10:11], in0=ss[:, 0:1], scalar1=1.0 / D,
                                                                     scalar2=EPS, op0=ALU.mult, op1=ALU.add),
                             reads=[kss], writes=[kss])
                        S.op("act", lambda e, ss=ss: e.activation(out=ss[:, 11:12], in_=ss[:, 10:11], func=AF.Sqrt),
                             reads=[kss], writes=[kss])
                        S.op("dve", lambda e, ss=ss: e.reciprocal(out=ss[:, 1:2], in_=ss[:, 11:12]),
                             reads=[kss], writes=[kss])
                        xs, kxs = A.carve("xs", HT_B + 57344 + t * 8192, 8192, BF16)
                        S.op("dve", lambda e, xs=xs, xt=xt, ss=ss: e.tensor_scalar(
                            out=xs, in0=xt, scalar1=ss[:, 1:2], scalar2=None, op0=ALU.mult), reads=[kx, kss], writes=[kxs])
                        xs_list.append((xs, kxs))
                    for kc in range(32):
                        tp, ktp = bank("tp", 4 + (kc % 4), 1, BF16)
                        for t in range(gn):
                            xs, kxs = xs_list[t]
                            S.op("pe", lambda e, tp=tp, xs=xs, t=t, kc=kc: e.transpose(
                                tp[:, t * 128:(t + 1) * 128], xs[:, kc * 128:(kc + 1) * 128], ident_b),
                                reads=[kxs, k_idb], writes=[ktp])
                        dst = hdst[:, kc, t_off + g0 * 128: t_off + (g0 + gn) * 128]
                        if kc % 2 == 0:
                            S.op("dve", lambda e, dst=dst, tp=tp, kc=kc, gn=gn: e.tensor_scalar(
                                out=dst, in0=tp[:, 0:gn * 128], scalar1=Acol[:, kc:kc + 1], scalar2=Bsel[:, kc],
                                op0=ALU.mult, op1=ALU.add), reads=[ktp, k_A1, k_A1c, k_mods], writes=[kdst])
                        else:
                            S.op("act", lambda e, dst=dst, tp=tp, kc=kc, gn=gn: e.activation(
                                out=dst, in_=tp[:, 0:gn * 128], func=AF.Identity, bias=Bsel[:, kc],
                                scale=Acol[:, kc:kc + 1]), reads=[ktp, k_A1, k_A1c, k_mods], writes=[kdst])

            B1 = mods3[:, 0:32, 0:1]
            B1c = mods3[:, 0:32, 1:2]

            hcT_raw, k_hcT = A.carve("hcT", HT_B, 16384, BF16)
            hcT = hcT_raw.rearrange("p (k t) -> p k t", t=CTX)
            hT_raw, k_hT = A.carve("hT", 0, HT_B, BF16)
            hT = hT_raw.rearrange("p (k t) -> p k t", t=1024)

            for hf in range(2):
                if hf == 0:
                    norm_transpose(ctx_d, 2, hcT, k_hcT, A1c, B1c, 0)
                norm_transpose(x_d[hf * 1024:(hf + 1) * 1024, :], 8, hT, k_hT, A1, B1, 0)
                if dbg and hf == 0:
                    S.op("sp", lambda e: e.dma_start(out=dbg_out["hT"], in_=hT_raw), reads=[k_hT], dma=True, is_out=True)
                if upto == "p1":
                    continue
                WA = HT_B + 16384
                for cg in range(96):
                    wst, kws = A.carve("wstA", WA + (cg % 2) * 16384, 16384)
                    qkeys = []
                    for q4 in range(4):
                        wq, kq = A.carve("wstAq", WA + (cg % 2) * 16384 + q4 * 4096, 4096)
                        S.op("sp", lambda e, wq=wq, cg=cg, q4=q4: e.dma_start(
                            out=wq.rearrange("p (k n) -> p k n", n=128),
                            in_=w_in_d[q4 * 1024:(q4 + 1) * 1024, cg * 128:(cg + 1) * 128].rearrange("(k p) n -> p k n", p=128)),
                            writes=[kq], dma=True)
                        qkeys.append(kq)
                    wb, kwb = A.carve("wbA", WA + 32768 + (cg % 3) * 8192, 8192, BF16)
                    S.op("pool", lambda e, wb=wb, wst=wst: e.tensor_copy(out=wb, in_=wst), reads=qkeys, writes=[kwb])
                    do_ctx = (hf == 0 and cg < 48)
                    pset = (cg % 2) * 4
                    pbs = [bank("pa", pset + i) for i in range(3 if do_ctx else 2)]
                    for kc in range(32):
                        for tb in range(2):
                            S.op("pe", lambda e, tb=tb, kc=kc, wb=wb, pb=pbs[tb][0]: e.matmul(
                                pb, wb[:, kc * 128:(kc + 1) * 128], hT[:, kc, tb * 512:(tb + 1) * 512],
                                start=(kc == 0), stop=(kc == 31)), reads=[kwb, k_hT], writes=[pbs[tb][1]])
                        if do_ctx:
                            S.op("pe", lambda e, kc=kc, wb=wb, pb=pbs[2][0]: e.matmul(
                                pb[:, 0:CTX], wb[:, kc * 128:(kc + 1) * 128], hcT[:, kc, :],
                                start=(kc == 0), stop=(kc == 31)), reads=[kwb, k_hcT], writes=[pbs[2][1]])
                    ost, kos = A.carve("ostA", WA + 57344 + (cg % 2) * 5120, 5120)
                    S.op("act", lambda e, ost=ost, pb=pbs[0][0]: e.copy(out=ost[:, 0:512], in_=pb),
                         reads=[pbs[0][1]], writes=[kos])
                    S.op("dve", lambda e, ost=ost, pb=pbs[1][0]: e.tensor_copy(out=ost[:, 512:1024], in_=pb),
                         reads=[pbs[1][1]], writes=[kos])
                    S.op("act", lambda e, ost=ost, cg=cg, hf=hf: e.dma_start(
                        out=projT_d[cg * 128:(cg + 1) * 128, hf * 1024:(hf + 1) * 1024], in_=ost[:, 0:1024]),
                        reads=[kos], writes=[("pj", cg)], dma=True)
                    if do_ctx:
                        S.op("dve", lambda e, ost=ost, pb=pbs[2][0]: e.tensor_copy(out=ost[:, 1024:1280], in_=pb[:, 0:CTX]),
                             reads=[pbs[2][1]], writes=[kos])
                        S.op("act", lambda e, ost=ost, cg=cg: e.dma_start(
                            out=cprojT_d[cg * 128:(cg + 1) * 128, :], in_=ost[:, 1024:1280]),
                            reads=[kos], writes=[("cpj", cg)], dma=True)


        AXX = mybir.AxisListType.X
        P1 = P0 + 4608
        lbp, k_lbp = A.carve("lbp", P1, 256)
        lbv, k_lbv = A.carve("lbv", P1 + 256, 128)
        oml, k_oml = A.carve("oml", P1 + 384, 128)
        hgnT, k_hgn = A.carve("hgnT", P1 + 512, 64)
        psclT, k_pscl = A.carve("psclT", P1 + 576, 64)
        mk, k_mk = A.carve("mk", P1 + 1024, 512)
        segm, k_segm = A.carve("segm", P1 + 1536, 8192)
        ones256, k_ones = A.carve("ones256", P1 + 9728, 1024)
        ones_b, k_onesb = A.carve("ones_b", P1 + 10752, 256, BF16)
        S0f, k_S0f = A.carve("S0f", P1 + 11008, 512)
        S0b, k_S0b = A.carve("S0b", P1 + 11520, 512)
        dec, k_dec = A.carve("dec", P1 + 12032, 128)
        decc, k_decc = A.carve("decc", P1 + 12160, 128)
        pmat, k_pmat = A.carve("pmat", P1 + 12288, 19 * 256, BF16)
        pmat_end = P1 + 12288 + 19 * 256

        def OP(eng, fn, reads, writes):
            S.op(eng, fn, reads=reads, writes=writes)

        def init_B_consts():
            ld(lbp, lbp_d, k_lbp)
            ld(hgnT, hgnT_d, k_hgn)
            ld(psclT, psclT_d, k_pscl)
            ld(mk, mk_d, k_mk)
            OP("pool", lambda e: e.memset(segm, 1.0), [], [k_segm])
            OP("pool", lambda e: e.memset(segm.rearrange("p (c j) -> p c j", j=64)[:, :, 0:1], 0.0), [], [k_segm])
            OP("pool", lambda e: e.memset(ones256, 1.0), [], [k_ones])
            OP("pool", lambda e: e.memset(ones_b, 1.0), [], [k_onesb])
            OP("dve", lambda e: e.tensor_tensor(out=oml, in0=lbp[:, 32:64], in1=lbp[:, 0:32], op=ALU.subtract), [k_lbp], [k_oml])
            OP("act", lambda e: e.activation(out=oml, in_=oml, func=AF.Exp), [k_oml], [k_oml])
            OP("dve", lambda e: e.tensor_scalar(out=oml, in0=oml, scalar1=1.0, scalar2=None, op0=ALU.add), [k_oml], [k_oml])
            OP("dve", lambda e: e.reciprocal(out=lbv, in_=oml), [k_oml], [k_lbv])
            OP("dve", lambda e: e.tensor_scalar(out=oml, in0=lbv, scalar1=-1.0, scalar2=1.0, op0=ALU.mult, op1=ALU.add), [k_lbv], [k_oml])


        def sigmoid_gate_f(dst, src, col, kdst, ksrc, eng2="dve"):
            OP("act", lambda e: e.activation(out=dst, in_=src, func=AF.Exp, scale=-1.0), [ksrc], [kdst])
            OP("dve", lambda e: e.tensor_scalar(out=dst, in0=dst, scalar1=1.0, scalar2=None, op0=ALU.add), [kdst], [kdst])
            OP("dve", lambda e: e.reciprocal(out=dst, in_=dst), [kdst], [kdst])
            OP("dve", lambda e: e.tensor_scalar(out=dst, in0=dst, scalar1=oml[:, col:col + 1], scalar2=lbv[:, col:col + 1],
                                                op0=ALU.mult, op1=ALU.add), [kdst, k_oml, k_lbv], [kdst])

        def transpose_to_tok(srcT, ksrc, dst_tok, kdst, ntiles, bank0):
            for g0 in range(0, ntiles, 4):
                gn = min(4, ntiles - g0)
                tp, ktp = bank("tpB", bank0 + (g0 // 4) % 2, 1, BF16)
                for t in range(gn):
                    OP("pe", lambda e, tp=tp, t=t, g0=g0: e.transpose(
                        tp[:, t * 128:(t + 1) * 128], srcT[:, (g0 + t) * 128:(g0 + t + 1) * 128], ident_b),
                        [ksrc, k_idb], [ktp])
                OP("act", lambda e, tp=tp, g0=g0, gn=gn: e.copy(
                    out=dst_tok[:, g0 * 128:(g0 + gn) * 128], in_=tp[:, 0:gn * 128]), [ktp], [kdst])

        def transpose_to_tok64(srcT, ksrc, dst_tok, kdst, nch, bank0):
            for g0 in range(0, nch, 8):
                gn = min(8, nch - g0)
                tp, ktp = bank("tpB64", bank0 + (g0 // 8) % 2, 1, BF16)
                for t in range(gn):
                    OP("pe", lambda e, tp=tp, t=t, g0=g0: e.transpose(
                        tp[0:64, t * 128:(t + 1) * 128], srcT[:, (g0 + t) * 64:(g0 + t + 1) * 64], ident_b),
                        [ksrc, k_idb], [ktp])
                OP("act", lambda e, tp=tp, g0=g0, gn=gn: e.copy(
                    out=dst_tok[0:64, g0 * 128:(g0 + gn) * 128], in_=tp[0:64, 0:gn * 128]), [ktp], [kdst])

        def phase_B_heads():
            K = 1024
            for h in range(nhb):
                ff, k_ff = A.carve("ff", 0, 8192)
                fb, k_fb = A.carve("fb", 8 * K, 8192)
                iT, k_iT = A.carve("iT", 16 * K, 8192)
                qT, k_qT = A.carve("qT", 24 * K, 8192)
                gT, k_gT = A.carve("gT", 32 * K, 8192)
                for (buf, kb, cg) in ((ff, k_ff, h), (fb, k_fb, 16 + h), (iT, k_iT, 32 + h), (qT, k_qT, 48 + h), (gT, k_gT, 64 + h)):
                    S.op("sp", lambda e, buf=buf, cg=cg: e.dma_start(out=buf, in_=projT_d[cg * 128:(cg + 1) * 128, :]),
                         reads=[("pj", cg)], writes=[kb], dma=True)
                cin, k_cin = A.carve("cin", 40 * K, 3072)
                for j, cg in enumerate((h, 16 + h, 32 + h)):
                    S.op("sp", lambda e, j=j, cg=cg: e.dma_start(out=cin[:, j * 256:(j + 1) * 256], in_=cprojT_d[cg * 128:(cg + 1) * 128, :]),
                         reads=[("cpj", cg)], writes=[k_cin], dma=True)
                if bstop == 1:
                    raise _Stop()
                if dbg and h == 0 and upto == "B":
                    dbg_out["din"] = dout("d_in", [128, 5 * 2048])
                    dbg_out["dsegm"] = dout("d_segm", [128, 2048])
                    for j, (buf, kb) in enumerate(((ff, k_ff), (fb, k_fb), (iT, k_iT), (qT, k_qT), (gT, k_gT))):
                        S.op("sp", lambda e, buf=buf, j=j: e.dma_start(out=dbg_out["din"][:, j * 2048:(j + 1) * 2048], in_=buf),
                             reads=[kb], dma=True, is_out=True)
                    S.op("sp", lambda e: e.dma_start(out=dbg_out["dsegm"], in_=segm), reads=[k_segm], dma=True, is_out=True)
                T1, k_T1 = A.carve("T1", 44 * K, 8192)
                T2, k_T2 = A.carve("T2", 52 * K, 8192)
                T3, k_T3 = A.carve("T3", 60 * K, 8192)
                cb16, k_cb16 = A.carve("cb16", 84 * K, 3 * 512, BF16)
                ctok, k_ctok = A.carve("ctok", 88 * K, 3 * 512, BF16)
                for d in range(2):
                    col = d * 16 + h
                    X = cin[:, d * 256:(d + 1) * 256]
                    f_ = T1[:, d * 256:(d + 1) * 256]
                    g_ = T2[:, d * 256:(d + 1) * 256]
                    cs = T3[:, d * 256:(d + 1) * 256]
                    sigmoid_gate_f(f_, X, col, k_T1, k_cin)
                    OP("act", lambda e, f_=f_, g_=g_: e.activation(out=g_, in_=f_, func=AF.Ln), [k_T1], [k_T2])
                    OP("dve", lambda e, cs=cs, g_=g_: e.tensor_tensor_scan(out=cs, data0=ones256, data1=g_, initial=0.0,
                                                                            op0=ALU.mult, op1=ALU.add), [k_T2, k_ones], [k_T3])
                    if d == 0:
                        OP("dve", lambda e, cs=cs: e.tensor_copy(out=small[:, 0:1], in_=cs[:, 255:256]), [k_T3], [k_small])
                        OP("dve", lambda e, cs=cs: e.tensor_scalar(out=cs, in0=cs, scalar1=-1.0, scalar2=small[:, 0:1],
                                                                     op0=ALU.mult, op1=ALU.add), [k_T3, k_small], [k_T3])
                    else:
                        OP("dve", lambda e, cs=cs, g_=g_: e.tensor_tensor(out=cs, in0=cs, in1=g_, op=ALU.subtract), [k_T3, k_T2], [k_T3])
                    OP("act", lambda e, cs=cs: e.activation(out=cs, in_=cs, func=AF.Exp), [k_T3], [k_T3])
                    OP("dve", lambda e, f_=f_: e.tensor_scalar(out=f_, in0=f_, scalar1=-1.0, scalar2=1.0, op0=ALU.mult, op1=ALU.add), [k_T1], [k_T1])
                    OP("dve", lambda e, f_=f_, cs=cs, d=d: e.tensor_tensor(out=cb16[:, d * 256:(d + 1) * 256], in0=f_, in1=cs, op=ALU.mult),
                       [k_T1, k_T3], [k_cb16])
                OP("act", lambda e: e.copy(out=cb16[:, 512:768], in_=cin[:, 512:768]), [k_cin], [k_cb16])
                transpose_to_tok(cb16, k_cb16, ctok, k_ctok, 6, 6)
                for d, (S0, kS0) in enumerate(((S0f, k_S0f), (S0b, k_S0b))):
                    sp_, ksp = bank("s0ps", 5)
                    for t in range(2):
                        OP("pe", lambda e, sp_=sp_, d=d, t=t: e.matmul(
                            sp_[:, 0:128], ctok[:, (d * 2 + t) * 128:(d * 2 + t + 1) * 128], ctok[:, (4 + t) * 128:(5 + t) * 128],
                            start=(t == 0), stop=(t == 1)), [k_ctok], [ksp])
                    OP("dve", lambda e, sp_=sp_, S0=S0: e.tensor_copy(out=S0, in_=sp_[:, 0:128]), [ksp], [kS0])
                if bstop == 2:
                    raise _Stop()

                Vb, k_Vb = A.carve("Vb", 112 * K, 4096, BF16)
                Vtok, k_Vtok = A.carve("Vtok", 104 * K, 8192, BF16)
                OP("act", lambda e: e.copy(out=Vb, in_=iT), [k_iT], [k_Vb])
                transpose_to_tok64(Vb, k_Vb, Vtok, k_Vtok, 32, 6)
                if bstop == 3:
                    raise _Stop()
                keep = {}
                for d in range(2):
                    col = d * 16 + h
                    X, kX = (ff, k_ff) if d == 0 else (fb, k_fb)
                    T1, k_T1 = A.carve("T1", 44 * K, 8192)
                    T2, k_T2 = A.carve("T2", 52 * K, 8192)
                    T3, k_T3 = A.carve("T3", 60 * K, 8192)
                    kf, k_kf = A.carve("kf", 68 * K, 8192)
                    cu, k_cu = A.carve("cu", 76 * K, 8192)
                    Qt, k_Qt = A.carve("Qt", 84 * K, 4096, BF16)
                    Kt, k_Kt = A.carve("Kt", 88 * K, 4096, BF16)
                    Kh, k_Kh = A.carve("Kh", 92 * K, 4096, BF16)
                    Khtok, k_Khtok = A.carve("Khtok", 96 * K, 8192, BF16)
                    Qh, k_Qh = A.carve("Qh", (132 + 4 * d) * K, 4096, BF16)
                    ATm, k_ATm = A.carve("ATm", (140 + 4 * d) * K, 4096, BF16)
                    Sb, k_Sb = A.carve("Sb", (148 + 8 * d) * K, 8192, BF16)
                    cu3 = cu.rearrange("p (c j) -> p c j", j=64)
                    sigmoid_gate_f(T1, X, col, k_T1, kX)
                    OP("act", lambda e, T1=T1, T2=T2: e.activation(out=T2, in_=T1, func=AF.Ln), [k_T1], [k_T2])
                    OP("pool", lambda e, T1=T1, kf=kf: e.tensor_scalar(out=kf, in0=T1, scalar1=-1.0, scalar2=1.0, op0=ALU.mult, op1=ALU.add), [k_T1], [k_kf])
                    OP("dve", lambda e, cu=cu, T2=T2: e.tensor_tensor_scan(out=cu, data0=segm, data1=T2, initial=0.0, op0=ALU.mult, op1=ALU.add),
                       [k_T2, k_segm], [k_cu])
                    if dbg and h == 0 and upto == "B":
                        dbg_out["dcu%d" % d] = dout("d_cu%d" % d, [128, 2048])
                        dbg_out["dg%d" % d] = dout("d_g%d" % d, [128, 2048])
                        S.op("sp", lambda e, cu=cu, d=d: e.dma_start(out=dbg_out["dcu%d" % d], in_=cu), reads=[k_cu], dma=True, is_out=True)
                        S.op("sp", lambda e, T2=T2, d=d: e.dma_start(out=dbg_out["dg%d" % d], in_=T2), reads=[k_T2], dma=True, is_out=True)
                    OP("dve", lambda e, cu3=cu3: e.tensor_copy(out=small[:, 32:64], in_=cu3[:, :, 63]), [k_cu], [k_small])
                    OP("act", lambda e: e.activation(out=dec, in_=small[:, 32:64], func=AF.Exp), [k_small], [k_dec])
                    if d == 1:
                        OP("dve", lambda e, T1=T1, cu=cu, T2=T2: e.tensor_tensor(out=T1, in0=T2, in1=cu, op=ALU.subtract), [k_T2, k_cu], [k_T1])
                        OP("dve", lambda e, T1=T1, cu=cu: e.tensor_tensor(
                            out=cu.rearrange("p (c j) -> p c j", j=64), in0=T1.rearrange("p (c j) -> p c j", j=64),
                            in1=small[:, 32:64].unsqueeze(2).to_broadcast([128, 32, 64]), op=ALU.add), [k_T1, k_small], [k_cu])
                    endi = 63 if d == 0 else 0
                    midi = 31 if d == 0 else 32
                    OP("act", lambda e, T3=T3, cu=cu: e.activation(out=T3, in_=cu, func=AF.Exp), [k_cu], [k_T3])
                    OP("pool", lambda e, Qh=Qh, T3=T3: e.tensor_tensor(out=Qh, in0=qT, in1=T3, op=ALU.mult), [k_qT, k_T3], [k_Qh])
                    OP("dve", lambda e, T1=T1, cu3=cu3, midi=midi: e.tensor_tensor(
                        out=T1.rearrange("p (c j) -> p c j", j=64), in0=cu3, in1=cu3[:, :, midi:midi + 1].to_broadcast([128, 32, 64]),
                        op=ALU.subtract), [k_cu], [k_T1])
                    OP("act", lambda e, T3=T3, T1=T1: e.activation(out=T3, in_=T1, func=AF.Exp), [k_T1], [k_T3])
                    OP("pool", lambda e, Qt=Qt, T3=T3: e.tensor_tensor(out=Qt, in0=qT, in1=T3, op=ALU.mult), [k_qT, k_T3], [k_Qt])
                    OP("act", lambda e, T2=T2, T1=T1: e.activation(out=T2, in_=T1, func=AF.Exp, scale=-1.0), [k_T1], [k_T2])
                    OP("pool", lambda e, Kt=Kt, T2=T2, kf=kf: e.tensor_tensor(out=Kt, in0=kf, in1=T2, op=ALU.mult), [k_kf, k_T2], [k_Kt])
                    OP("dve", lambda e, T1=T1, cu3=cu3, endi=endi: e.tensor_tensor(
                        out=T1.rearrange("p (c j) -> p c j", j=64), in0=cu3[:, :, endi:endi + 1].to_broadcast([128, 32, 64]), in1=cu3,
                        op=ALU.subtract), [k_cu], [k_T1])
                    OP("act", lambda e, T3=T3, T1=T1: e.activation(out=T3, in_=T1, func=AF.Exp), [k_T1], [k_T3])
                    OP("dve", lambda e, Kh=Kh, T3=T3, kf=kf: e.tensor_tensor(out=Kh, in0=kf, in1=T3, op=ALU.mult), [k_kf, k_T3], [k_Kh])
                    if bstop == 4:
                        raise _Stop()
                    transpose_to_tok64(Kh, k_Kh, Khtok, k_Khtok, 32, 6)
                    if bstop == 5:
                        raise _Stop()
                    Ubuf, k_U = A.carve("Ubuf", 116 * K, 16384)
                    for c0 in range(0, 32, 4):
                        up, kup = bank("ups", (c0 // 4) % 2 + 4)
                        for c in range(c0, c0 + 4):
                            OP("pe", lambda e, up=up, c=c, c0=c0: e.matmul(
                                up[:, (c - c0) * 128:(c - c0 + 1) * 128],
                                Khtok[0:64, c * 128:(c + 1) * 128], Vtok[0:64, c * 128:(c + 1) * 128],
                                start=True, stop=True), [k_Khtok, k_Vtok], [kup])
                        if d == 0:
                            OP("act", lambda e, up=up, c0=c0: e.copy(out=Ubuf[:, c0 * 128:(c0 + 4) * 128], in_=up), [kup], [k_U])
                        else:
                            for c in range(c0, c0 + 4):
                                OP("act", lambda e, up=up, c=c, c0=c0: e.copy(
                                    out=Ubuf[:, (31 - c) * 128:(32 - c) * 128], in_=up[:, (c - c0) * 128:(c - c0 + 1) * 128]), [kup], [k_U])
                    if bstop == 6:
                        raise _Stop()
                    S0, kS0 = (S0f, k_S0f) if d == 0 else (S0b, k_S0b)
                    if d == 0:
                        OP("dve", lambda e: e.tensor_copy(out=decc, in_=dec), [k_dec], [k_decc])
                    else:
                        for c in range(32):
                            OP("pool", lambda e, c=c: e.tensor_copy(out=decc[:, 31 - c:32 - c], in_=dec[:, c:c + 1]), [k_dec], [k_decc])
                    SA0, k_SA0 = A.carve("SallLo", 0, 8192)
                    SA1, k_SA1 = A.carve("SallHi", 16 * K, 8192)

                    def sl(st):
                        return (SA0, k_SA0, st) if st < 16 else (SA1, k_SA1, st - 16)
                    for st in range(32):
                        buf, kbuf, j = sl(st)
                        if st == 0:
                            prev, kprev = S0, kS0
                        else:
                            pb, kpb, pj = sl(st - 1)
                            prev, kprev = pb[:, pj * 128:(pj + 1) * 128], kpb
                        OP("dve", lambda e, st=st, prev=prev, buf=buf, j=j, Ubuf=Ubuf: e.scalar_tensor_tensor(
                            out=buf[:, j * 128:(j + 1) * 128], in0=prev, scalar=decc[:, st:st + 1], in1=Ubuf[:, st * 128:(st + 1) * 128],
                            op0=ALU.mult, op1=ALU.add), [kprev, k_decc, k_U, kbuf], [kbuf])
                    OP("act", lambda e, Sb=Sb, S0=S0: e.copy(out=Sb[:, 0:128], in_=S0), [kS0], [k_Sb])
                    OP("act", lambda e, Sb=Sb, SA0=SA0: e.copy(out=Sb[:, 128:17 * 128], in_=SA0), [k_SA0], [k_Sb])
                    OP("act", lambda e, Sb=Sb, SA1=SA1: e.copy(out=Sb[:, 17 * 128:32 * 128], in_=SA1[:, 0:15 * 128]), [k_SA1], [k_Sb])
                    if bstop == 7:
                        raise _Stop()
                    AT3 = ATm.rearrange("p (c t) -> p c t", t=64)
                    KtZ, k_KtZ = A.carve("KtZ", 60 * K, 4096, BF16)
                    OP("pool", lambda e, KtZ=KtZ, Kt=Kt: e.tensor_copy(out=KtZ, in_=Kt), [k_Kt], [k_KtZ])
                    zlo = 32 if d == 0 else 0
                    OP("pool", lambda e, KtZ=KtZ, zlo=zlo: e.memset(KtZ.rearrange("p (c j) -> p c j", j=64)[:, :, zlo:zlo + 32], 0.0), [], [k_KtZ])
                    for c0 in range(0, 32, 8):
                        ap_, kap = bank("atps", (c0 // 8) % 2 + 6)
                        for c in range(c0, c0 + 8):
                            KL, kKL = (KtZ, k_KtZ) if d == 0 else (Kt, k_Kt)
                            KR, kKR = (Kt, k_Kt) if d == 0 else (KtZ, k_KtZ)
                            OP("pe", lambda e, ap_=ap_, c=c, c0=c0, KL=KL, Qt=Qt: e.matmul(
                                ap_[0:64, (c - c0) * 64:(c - c0) * 64 + 32], KL[:, c * 64:(c + 1) * 64], Qt[:, c * 64:c * 64 + 32],
                                start=True, stop=True), [kKL, k_Qt], [kap])
                            OP("pe", lambda e, ap_=ap_, c=c, c0=c0, KR=KR, Qt=Qt: e.matmul(
                                ap_[0:64, (c - c0) * 64 + 32:(c - c0 + 1) * 64], KR[:, c * 64:(c + 1) * 64], Qt[:, c * 64 + 32:(c + 1) * 64],
                                start=True, stop=True), [kKR, k_Qt], [kap])
                        ap3 = ap_.rearrange("p (c t) -> p c t", t=64)
                        OP("dve", lambda e, ap3=ap3, AT3=AT3, c0=c0, d=d: e.tensor_tensor(
                            out=AT3[0:64, c0:c0 + 8, :], in0=ap3[0:64, :, :],
                            in1=mk[0:64, d * 64:(d + 1) * 64].unsqueeze(1).to_broadcast([64, 8, 64]), op=ALU.mult),
                            [kap, k_mk], [k_ATm])
                    keep[d] = (Qh, k_Qh, ATm, k_ATm, Sb, k_Sb)
                if bstop == 8:
                    raise _Stop()
                sg, k_sg = A.carve("sg", 60 * K, 8192)
                OP("act", lambda e: e.activation(out=sg, in_=gT, func=AF.Exp, scale=-1.0), [k_gT], [k_sg])
                OP("dve", lambda e: e.tensor_scalar(out=sg, in0=sg, scalar1=1.0, scalar2=None, op0=ALU.add), [k_sg], [k_sg])
                OP("dve", lambda e: e.reciprocal(out=sg, in_=sg), [k_sg], [k_sg])
                sgb, k_sgb = A.carve("sgb", 164 * K, 4096, BF16)
                OP("pool", lambda e: e.tensor_tensor(out=sgb, in0=sg, in1=gT, op=ALU.mult), [k_sg, k_gT], [k_sgb])
                mixo, k_mixo = A.carve("mixo", 112 * K, 4096, BF16)
                for tb in range(4):
                    op_, kop = bank("ops", tb % 2)
                    for cl in range(8):
                        c = tb * 8 + cl
                        tile_, r0 = c // 2, (c % 2) * 64
                        n = 0
                        for d in range(2):
                            Qh, k_Qh, ATm, k_ATm, Sb, k_Sb = keep[d]
                            s = c if d == 0 else 31 - c
                            OP("pe", lambda e, op_=op_, cl=cl, ATm=ATm, c=c, n=n: e.matmul(
                                op_[:, cl * 64:(cl + 1) * 64], Vtok[0:64, c * 128:(c + 1) * 128],
                                ATm[0:64, c * 64:(c + 1) * 64], start=(n == 0), stop=False), [k_Vtok, k_ATm], [kop])
                            n += 1
                            OP("pe", lambda e, op_=op_, cl=cl, Sb=Sb, s=s, Qh=Qh, c=c, n=n: e.matmul(
                                op_[:, cl * 64:(cl + 1) * 64], Sb[:, s * 128:(s + 1) * 128], Qh[:, c * 64:(c + 1) * 64],
                                start=False, stop=(n == 3)), [k_Sb, k_Qh], [kop])
                            n += 1
                    sq, k_sq = A.carve("sq", 116 * K + (tb % 2) * 1024, 1024, BF16)
                    OP("act", lambda e, sq=sq, op_=op_: e.activation(out=sq, in_=op_, func=AF.Square), [kop], [k_sq])
                    np_, knp = bank("nps", 2 + tb % 2)
                    OP("pe", lambda e, np_=np_, sq=sq: e.matmul(np_, ones_b, sq, start=True, stop=True), [k_onesb, k_sq], [knp])
                    rs, k_rs = A.carve("rs", 120 * K + (tb % 2) * 2048, 2048)
                    OP("dve", lambda e, rs=rs, np_=np_: e.tensor_scalar(out=rs, in0=np_, scalar1=1.0 / 128, scalar2=EPS, op0=ALU.mult, op1=ALU.add),
                       [knp], [k_rs])
                    OP("act", lambda e, rs=rs: e.activation(out=rs, in_=rs, func=AF.Sqrt), [k_rs], [k_rs])
                    OP("dve", lambda e, rs=rs: e.reciprocal(out=rs, in_=rs), [k_rs], [k_rs])
                    OP("dve", lambda e, rs=rs, op_=op_: e.tensor_tensor(out=rs, in0=op_, in1=rs, op=ALU.mult), [kop, k_rs], [k_rs])
                    OP("dve", lambda e, rs=rs, tb=tb, h=h: e.scalar_tensor_tensor(
                        out=mixo[:, tb * 512:(tb + 1) * 512], in0=rs, scalar=hgnT[:, h:h + 1], in1=sgb[:, tb * 512:(tb + 1) * 512],
                        op0=ALU.mult, op1=ALU.mult), [k_rs, k_hgn, k_sgb], [k_mixo])
                S.op("act", lambda e, h=h: e.dma_start(out=mixT_d[h * 128:(h + 1) * 128, :], in_=mixo),
                     reads=[k_mixo], writes=[("mx", h)], dma=True)
                if dbg and h == 0 and upto == "B":
                    dbg_out["dmixo"] = dout("d_mixo", [128, 2048], BF16)
                    S.op("act", lambda e: e.dma_start(out=dbg_out["dmixo"], in_=mixo), reads=[k_mixo], dma=True, is_out=True)
                if HEAD_BARRIER:
                    S.barrier()

        def phase_B_pools():
            K = 1024
            pst, k_pst = A.carve("pst", 0, 19 * 512)
            ld(pst, pmat_d, k_pst)
            OP("pool", lambda e: e.tensor_copy(out=pmat, in_=pst), [k_pst], [k_pmat])
            OFFS = {0: (-1, 0), 1: (-1, 0, 1), 2: (-2, -1, 0, 1, 2), 3: (-4, -3, -2, -1, 0, 1, 2, 3, 4)}
            mbase = {0: 0, 1: 2, 2: 5, 3: 10}
            for gi in range(npool):
                S.barrier()
                vT, k_vT = A.carve("vT", 0, 32768)
                vb, k_vb = A.carve("vb", 32 * K, 16384, BF16)
                vtok, k_vtok = A.carve("vtok", 48 * K, 16384, BF16)
                icb, k_icb = A.carve("icb", 64 * K, 8192)
                dif, k_dif = A.carve("dif", 72 * K, 16384, BF16)
                pws, k_pws = A.carve("pws", 88 * K, 8192)
                pwb, k_pwb = A.carve("pwb", 96 * K, 4096, BF16)
                ld(icb, icnt_d[gi], k_icb)
                S.op("sp", lambda e, gi=gi: e.dma_start(out=pws.rearrange("p (j n) -> p j n", n=512),
                                                         in_=pool_w_d[gi].rearrange("(j p) n -> p j n", p=128)), writes=[k_pws], dma=True)
                OP("pool", lambda e: e.tensor_copy(out=pwb, in_=pws), [k_pws], [k_pwb])
                for j in range(4):
                    cg = 80 + gi * 4 + j
                    S.op("sp", lambda e, j=j, cg=cg: e.dma_start(out=vT[:, j * 2048:(j + 1) * 2048], in_=projT_d[cg * 128:(cg + 1) * 128, :]),
                         reads=[("pj", cg)], writes=[k_vT], dma=True)
                OP("act", lambda e: e.copy(out=vb, in_=vT), [k_vT], [k_vb])
                for j in range(4):
                    transpose_to_tok(vb[:, j * 2048:(j + 1) * 2048], k_vb, vtok[:, j * 2048:(j + 1) * 2048], k_vtok, 16, 6)
                for j in range(4):
                    for tb in range(4):
                        dp, kdp = bank("dps", (j * 4 + tb) % 2 + 4)
                        for tl in range(4):
                            T_ = tb * 4 + tl
                            offs = [o for o in OFFS[gi] if 0 <= T_ + o <= 15]
                            for n, o in enumerate(offs):
                                mi = mbase[gi] + OFFS[gi].index(o)
                                OP("pe", lambda e, dp=dp, tl=tl, j=j, T_=T_, o=o, mi=mi, n=n, offs=offs: e.matmul(
                                    dp[:, tl * 128:(tl + 1) * 128], vtok[:, j * 2048 + (T_ + o) * 128: j * 2048 + (T_ + o + 1) * 128],
                                    pmat[:, mi * 128:(mi + 1) * 128], start=(n == 0), stop=(n == len(offs) - 1)), [k_vtok, k_pmat], [kdp])
                        tmp, k_tmp = A.carve("ptmp", 100 * K + ((j * 4 + tb) % 2) * 2048, 2048)
                        OP("dve", lambda e, tmp=tmp, dp=dp, tb=tb: e.tensor_tensor(out=tmp, in0=dp, in1=icb[:, tb * 512:(tb + 1) * 512], op=ALU.mult),
                           [kdp, k_icb], [k_tmp])
                        OP("dve", lambda e, tmp=tmp, j=j, tb=tb: e.tensor_tensor(
                            out=dif[:, j * 2048 + tb * 512: j * 2048 + (tb + 1) * 512], in0=tmp,
                            in1=vT[:, j * 2048 + tb * 512: j * 2048 + (tb + 1) * 512], op=ALU.subtract), [k_tmp, k_vT], [k_dif])
                for jo in range(4):
                    mixo, k_mixo = A.carve("mixo", 104 * K + (jo % 2) * 4096, 4096, BF16)
                    for tb in range(4):
                        pp, kpp = bank("pps", (jo * 4 + tb) % 2 + 2)
                        for j in range(4):
                            OP("pe", lambda e, pp=pp, j=j, jo=jo, tb=tb: e.matmul(
                                pp, pwb[:, j * 512 + jo * 128: j * 512 + (jo + 1) * 128],
                                dif[:, j * 2048 + tb * 512: j * 2048 + (tb + 1) * 512], start=(j == 0), stop=(j == 3)), [k_pwb, k_dif], [kpp])
                        OP("act", lambda e, pp=pp, mixo=mixo, tb=tb, gi=gi, jo=jo: e.activation(
                            out=mixo[:, tb * 512:(tb + 1) * 512], in_=pp, func=AF.Identity, scale=psclT[:, gi * 4 + jo: gi * 4 + jo + 1]),
                            [kpp, k_pscl], [k_mixo])
                    row = 16 + gi * 4 + jo
                    S.op("act", lambda e, row=row, mixo=mixo: e.dma_start(out=mixT_d[row * 128:(row + 1) * 128, :], in_=mixo),
                         reads=[k_mixo], writes=[("mx", row)], dma=True)

        def phase_B():
            phase_B_pools()
            S.barrier()
            phase_B_heads()

        P2 = pmat_end
        ones_f, k_onesf = A.carve("ones_f", P2, 512)
        n2gT, k_n2g = A.carve("n2gT", P2 + 512, 128)
        A2, k_A2 = A.carve("A2", P2 + 640, 128)
        ssq2, k_ssq2 = A.carve("ssq2", P2 + 768, 512)
        rstd2, k_rstd2 = A.carve("rstd2", P2 + 1280, 64)
        iota256, k_iota = A.carve("iota256", P2 + 1344, 1024)
        tokidx, k_tokidx = A.carve("tokidx", P2 + 2368, 64)
        idx_i, k_idx = A.carve("idx_i", P2 + 2432, 128, I32)
        gate, k_gate = A.carve("gate", P2 + 2560, 128)
        idx_f, k_idxf = A.carve("idx_f", P2 + 2688, 128)
        fss, k_fss = A.carve("fss", P2 + 2816, 64)
        assert P2 + 2880 <= SB_BYTES

        def bcast_row(col, kcol, dst, kdst):
            for g0 in range(0, 32, 4):
                bp, kbp = bank("bcps", (g0 // 4) % 2 + 6)
                for i in range(4):
                    kc = g0 + i
                    dg, kdg = A.carve("diag", P2 + 2880 + (kc % 2) * 512, 512)
                    OP("dve", lambda e, dg=dg, kc=kc: e.tensor_scalar(out=dg, in0=ident_f, scalar1=col[:, kc:kc + 1], scalar2=None, op0=ALU.mult),
                       [k_idf, kcol], [kdg])
                    OP("pe", lambda e, bp=bp, i=i, dg=dg: e.matmul(bp[:, i * 128:(i + 1) * 128], ones_f, dg, start=True, stop=True),
                       [k_onesf, kdg], [kbp])
                OP("act", lambda e, bp=bp, g0=g0: e.copy(out=dst[:, g0 * 128:(g0 + 4) * 128], in_=bp), [kbp], [kdst])

        def phase_C():
            K = 1024
            ld(n2gT, n2gT_d, k_n2g)
            OP("pool", lambda e: e.memset(ones_f, 1.0), [], [k_onesf])
            OP("pool", lambda e: e.memset(ssq2, 0.0), [], [k_ssq2])
            G1b, k_G1b = A.carve("G1b", 144 * K, 16384)
            g1col, k_g1c = A.carve("g1col", P2 + 2880 + 1024, 128)
            OP("dve", lambda e: e.tensor_copy(out=g1col, in_=mods3[:, 64:96, 0]), [k_mods], [k_g1c])
            bcast_row(g1col, k_g1c, G1b, k_G1b)
            for hf in range(2):
                mixh = []
                for kc in range(32):
                    mb, kmb = A.carve("mixh", kc * 2048, 2048, BF16)
                    S.op("sp", lambda e, mb=mb, kc=kc, hf=hf: e.dma_start(out=mb, in_=mixT_d[kc * 128:(kc + 1) * 128, hf * 1024:(hf + 1) * 1024]),
                         reads=[("mx", kc)], writes=[kmb], dma=True)
                    mixh.append((mb, kmb))
                for cb in range(8):
                    wbs = []
                    for pc in range(8):
                        st, kst = A.carve("wstC", 128 * K + (pc % 2) * 8192, 8192)
                        S.op("sp", lambda e, st=st, pc=pc, cb=cb: e.dma_start(
                            out=st.rearrange("p (k n) -> p k n", n=512),
                            in_=w_out_d[pc * 512:(pc + 1) * 512, cb * 512:(cb + 1) * 512].rearrange("(k p) n -> p k n", p=128)),
                            writes=[kst], dma=True)
                        wb, kwb = A.carve("wbC", 64 * K + (cb % 2) * 32768 + pc * 4096, 4096, BF16)
                        OP("pool", lambda e, wb=wb, st=st: e.tensor_copy(out=wb, in_=st), [kst], [kwb])
                        wbs.append((wb, kwb))
                    for tt in range(8):
                        ps_, kps = bank("cps", tt)
                        for kc in range(32):
                            wb, kwb = wbs[kc // 4]
                            mb, kmb = mixh[kc]
                            OP("pe", lambda e, ps_=ps_, mb=mb, wb=wb, kc=kc, tt=tt: e.matmul(
                                ps_, mb[:, tt * 128:(tt + 1) * 128], wb[:, (kc % 4) * 512:(kc % 4 + 1) * 512],
                                start=(kc == 0), stop=(kc == 31)), [kmb, kwb], [kps])
                        gt = hf * 8 + tt
                        xt, kxt = A.carve("xtC", 160 * K + (tt % 2) * 2048, 2048)
                        S.op("sp", lambda e, xt=xt, gt=gt, cb=cb: e.dma_start(out=xt, in_=x_d[gt * 128:(gt + 1) * 128, cb * 512:(cb + 1) * 512]),
                             writes=[kxt], dma=True)
                        xo, kxo = A.carve("xoC", 164 * K + (tt % 2) * 2048, 2048)
                        OP("dve", lambda e, xo=xo, ps_=ps_, cb=cb: e.tensor_tensor(out=xo, in0=ps_, in1=G1b[:, cb * 512:(cb + 1) * 512], op=ALU.mult),
                           [kps, k_G1b], [kxo])
                        OP("dve", lambda e, xo=xo, xt=xt: e.tensor_tensor(out=xo, in0=xo, in1=xt, op=ALU.add), [kxo, kxt], [kxo])
                        OP("act", lambda e, xt=xt, xo=xo, gt=gt, cb=cb: e.activation(
                            out=xt, in_=xo, func=AF.Square, accum_out=ssq2[:, gt * 8 + cb: gt * 8 + cb + 1]), [kxo, k_ssq2], [kxt, k_ssq2])
                        S.op("act", lambda e, xo=xo, gt=gt, cb=cb: e.dma_start(out=x1_d[gt * 128:(gt + 1) * 128, cb * 512:(cb + 1) * 512], in_=xo),
                             reads=[kxo], writes=[("x1", gt)], dma=True)

        afft_g = []

        def phase_D():
            K = 1024
            ld(iota256, iota_d, k_iota)
            ld(tokidx, tokidx_d, k_tokidx)
            OP("dve", lambda e: e.reduce_sum(out=rstd2, in_=ssq2.rearrange("p (t c) -> p t c", c=8), axis=AXX), [k_ssq2], [k_rstd2])
            OP("dve", lambda e: e.tensor_scalar(out=rstd2, in0=rstd2, scalar1=1.0 / D, scalar2=EPS, op0=ALU.mult, op1=ALU.add), [k_rstd2], [k_rstd2])
            OP("act", lambda e: e.activation(out=rstd2, in_=rstd2, func=AF.Sqrt), [k_rstd2], [k_rstd2])
            OP("dve", lambda e: e.reciprocal(out=rstd2, in_=rstd2), [k_rstd2], [k_rstd2])
            OP("dve", lambda e: e.scalar_tensor_tensor(out=A2, in0=mods3[:, 128:160, 0], scalar=1.0, in1=n2gT, op0=ALU.add, op1=ALU.mult),
               [k_mods, k_n2g], [k_A2])
            A2b, k_A2b = A.carve("A2b", 32 * K, 16384)
            B2b, k_B2b = A.carve("B2b", 48 * K, 16384)
            b2col, k_b2c = A.carve("b2col", P2 + 2880 + 1024, 128)
            OP("dve", lambda e: e.tensor_copy(out=b2col, in_=mods3[:, 96:128, 0]), [k_mods], [k_b2c])
            bcast_row(A2, k_A2, A2b, k_A2b)
            bcast_row(b2col, k_b2c, B2b, k_B2b)
            Rw, k_Rw = A.carve("Rw", 96 * K, 2048)
            ld(Rw, rwT_d, k_Rw)
            afft, k_afft = A.carve("afft", 98 * K, 1024)
            valT, k_valT = A.carve("valT", 99 * K, 1024)
            afft_g.extend([afft, k_afft, valT, k_valT])
            R2, k_R2 = A.carve("R2", 100 * K, 2048)
            affT, k_affT = A.carve("affT", 104 * K, 8192, parts=16)
            wk, k_wk = A.carve("wk", 112 * K, 8192, parts=16)
            msk, k_msk = A.carve("msk", 120 * K, 8192, parts=16)
            onesk, k_onesk = A.carve("onesk", 128 * K, 8192, parts=16)
            mx8, k_mx8 = A.carve("mx8", 136 * K, 32, parts=16)
            h2T, k_h2T = A.carve("h2T", 80 * K, 16384)
            sm, k_sm = A.carve("smx", 136 * K + 64, 64)
            for tt in range(16):
                x1t, kx1 = A.carve("x1t", (tt % 2) * 16384, 16384)
                S.op("sp", lambda e, x1t=x1t, tt=tt: e.dma_start(out=x1t, in_=x1_d[tt * 128:(tt + 1) * 128, :]),
                     reads=[("x1", tt)], writes=[kx1], dma=True)
                OP("dve", lambda e, x1t=x1t, tt=tt: e.scalar_tensor_tensor(out=x1t, in0=x1t, scalar=rstd2[:, tt:tt + 1], in1=A2b,
                                                                           op0=ALU.mult, op1=ALU.mult), [kx1, k_rstd2, k_A2b], [kx1])
                OP("pool", lambda e, x1t=x1t: e.tensor_tensor(out=x1t, in0=x1t, in1=B2b, op=ALU.add), [kx1, k_B2b], [kx1])
                h2b, kh2b = A.carve("h2b", 64 * K + (tt % 2) * 8192, 8192, BF16)
                OP("act", lambda e, h2b=h2b, x1t=x1t: e.copy(out=h2b, in_=x1t), [kx1], [kh2b])
                S.op("act", lambda e, h2b=h2b, tt=tt: e.dma_start(out=h2_d[tt * 128:(tt + 1) * 128, :], in_=h2b),
                     reads=[kh2b], writes=[("h2d", tt)], dma=True)
                for g0 in range(0, 32, 4):
                    tp, ktp = bank("tpD", (g0 // 4) % 2 + 4)
                    for i in range(4):
                        kc = g0 + i
                        OP("pe", lambda e, tp=tp, i=i, kc=kc, x1t=x1t: e.transpose(tp[:, i * 128:(i + 1) * 128], x1t[:, kc * 128:(kc + 1) * 128], ident_f),
                           [kx1, k_idf], [ktp])
                    eng = "act" if (g0 // 4) % 2 == 0 else "dve"
                    if eng == "act":
                        OP("act", lambda e, tp=tp, g0=g0: e.copy(out=h2T[:, g0 * 128:(g0 + 4) * 128], in_=tp), [ktp], [k_h2T])
                    else:
                        OP("dve", lambda e, tp=tp, g0=g0: e.tensor_copy(out=h2T[:, g0 * 128:(g0 + 4) * 128], in_=tp), [ktp], [k_h2T])
                lg, klg = bank("lgps", 6)
                for kc in range(32):
                    OP("pe", lambda e, lg=lg, kc=kc: e.matmul(lg[:, 0:16], h2T[:, kc * 128:(kc + 1) * 128], Rw[:, kc * 16:(kc + 1) * 16],
                                                              start=(kc == 0), stop=(kc == 31)), [k_h2T, k_Rw], [klg])
                OP("dve", lambda e, lg=lg: e.reduce_max(out=sm[:, 0:1], in_=lg[:, 0:16], axis=AXX), [klg], [k_sm])
                OP("dve", lambda e: e.tensor_scalar(out=sm[:, 1:2], in0=sm[:, 0:1], scalar1=-1.0, scalar2=None, op0=ALU.mult), [k_sm], [k_sm])
                OP("pool", lambda e: e.memset(sm[:, 2:3], 0.0), [], [k_sm])
                OP("act", lambda e, lg=lg, tt=tt: e.activation(out=afft[:, tt * 16:(tt + 1) * 16], in_=lg[:, 0:16], func=AF.Exp, bias=sm[:, 1:2],
                                                               scale=1.0, accum_out=sm[:, 2:3]), [klg, k_sm], [k_afft, k_sm])
                OP("dve", lambda e: e.reciprocal(out=sm[:, 3:4], in_=sm[:, 2:3]), [k_sm], [k_sm])
                OP("dve", lambda e, tt=tt: e.tensor_scalar(out=afft[:, tt * 16:(tt + 1) * 16], in0=afft[:, tt * 16:(tt + 1) * 16], scalar1=sm[:, 3:4],
                                                           scalar2=None, op0=ALU.mult), [k_afft, k_sm], [k_afft])
                t2, kt2 = bank("t2ps", 7)
                OP("pe", lambda e, t2=t2, tt=tt: e.transpose(t2[0:16, 0:128], afft[:, tt * 16:(tt + 1) * 16], ident_f), [k_afft, k_idf], [kt2])
                OP("act", lambda e, t2=t2, tt=tt: e.copy(out=affT[:, tt * 128:(tt + 1) * 128], in_=t2[0:16, 0:128]), [kt2], [k_affT])
            OP("dve", lambda e: e.tensor_copy(out=wk, in_=affT), [k_affT], [k_wk])
            OP("pool", lambda e: e.memset(onesk, 1.0), [], [k_onesk])
            for r in range(CAP // 8):
                OP("dve", lambda e: e.max(out=mx8, in_=wk), [k_wk], [k_mx8])
                if r < CAP // 8 - 1:
                    OP("dve", lambda e: e.match_replace(out=wk, in_to_replace=mx8, in_values=wk, imm_value=-1.0), [k_wk, k_mx8], [k_wk])
            OP("dve", lambda e: e.tensor_scalar(out=msk, in0=affT, scalar1=mx8[:, 7:8], scalar2=None, op0=ALU.is_ge), [k_affT, k_mx8], [k_msk])
            OP("dve", lambda e: e.tensor_tensor_scan(out=wk, data0=onesk, data1=msk, initial=0.0, op0=ALU.mult, op1=ALU.add), [k_onesk, k_msk], [k_wk])
            OP("dve", lambda e: e.tensor_tensor(out=wk, in0=wk, in1=msk, op=ALU.mult), [k_wk, k_msk], [k_wk])
            for tt in range(16):
                t2, kt2 = bank("t2ps", 7)
                OP("pe", lambda e, t2=t2, tt=tt: e.transpose(t2[:, 0:16], wk[:, tt * 128:(tt + 1) * 128], ident_f[0:16, 0:16]), [k_wk, k_idf], [kt2])
                OP("act", lambda e, t2=t2, tt=tt: e.copy(out=valT[:, tt * 16:(tt + 1) * 16], in_=t2[:, 0:16]), [kt2], [k_valT])
            R2v = R2.rearrange("p (t e two) -> p t e two", e=16, two=2)
            OP("dve", lambda e: e.tensor_copy(out=R2v[:, :, :, 1], in_=afft.rearrange("p (t e) -> p t e", e=16)), [k_afft], [k_R2])
            OP("dve", lambda e: e.tensor_copy(out=R2v[:, :, :, 0], in_=tokidx.unsqueeze(2).to_broadcast([128, 16, 16])), [k_tokidx], [k_R2])
            ig0, kig0 = bank("ig0", 4)
            ig1, kig1 = bank("ig1", 5)
            igs = ((ig0, kig0), (ig1, kig1))
            for ex in range(NE):
                for tt in range(16):
                    sel, ksel = A.carve("sel", 102 * K + (tt % 2) * 1024, 1024)
                    OP("dve", lambda e, sel=sel, tt=tt, ex=ex: e.tensor_scalar(out=sel, in0=iota256, scalar1=valT[:, tt * 16 + ex: tt * 16 + ex + 1],
                                                                               scalar2=None, op0=ALU.is_equal), [k_iota, k_valT], [ksel])
                    for jh in range(2):
                        ig, kig = igs[jh]
                        OP("pe", lambda e, ig=ig, sel=sel, jh=jh, tt=tt, ex=ex: e.matmul(
                            ig[:, ex * 2:ex * 2 + 2], sel[:, jh * 128:(jh + 1) * 128], R2[:, (tt * 16 + ex) * 2:(tt * 16 + ex) * 2 + 2],
                            start=(tt == 0), stop=(tt == 15)), [ksel, k_R2], [kig])
            for jh in range(2):
                ig, kig = igs[jh]
                igv = ig[:, 0:32].rearrange("p (e two) -> p e two", two=2)
                OP("dve", lambda e, igv=igv, jh=jh: e.tensor_copy(out=idx_f.rearrange("p (e j) -> p e j", j=2)[:, :, jh], in_=igv[:, :, 0]), [kig], [k_idxf])
                OP("dve", lambda e, igv=igv, jh=jh: e.tensor_copy(out=gate.rearrange("p (e j) -> p e j", j=2)[:, :, jh], in_=igv[:, :, 1]), [kig], [k_gate])
            OP("dve", lambda e: e.tensor_copy(out=idx_i, in_=idx_f), [k_idxf], [k_idx])

        def phase_E():
            K = 1024
            import concourse.bass as _b
            h2keys = [("h2d", tt) for tt in range(16)]
            zt, kzt = A.carve("zt", 146 * K, 8192)
            OP("pool", lambda e: e.memset(zt, 0.0), [], [kzt])
            for dh in range(2):
                for tt in range(16):
                    S.op("sp", lambda e, dh=dh, tt=tt: e.dma_start(out=moe_d[dh][tt * 128:(tt + 1) * 128, :], in_=zt),
                         reads=[kzt], writes=[("moe", dh)], dma=True)
            ncast = [0]

            def cast(dst, src, ksrc, kdst):
                eng = ("pool", "dve", "act")[ncast[0] % 3]
                ncast[0] += 1
                if eng == "act":
                    OP("act", lambda e: e.copy(out=dst, in_=src), [ksrc], [kdst])
                else:
                    OP(eng, lambda e: e.tensor_copy(out=dst, in_=src), [ksrc], [kdst])

            for ex in range(NE):
                S.barrier()
                xgT, k_xgT = A.carve("xgT", 16 * K, 16384, BF16)
                for jh in range(2):
                    col = ex * 2 + jh
                    xg, kxg = A.carve("xg", jh * 8192, 8192, BF16)
                    S.op("pool", lambda e, xg=xg, col=col: e.indirect_dma_start(
                        out=xg, out_offset=None, in_=h2_d[:, :], in_offset=_b.IndirectOffsetOnAxis(ap=idx_i[:, col:col + 1], axis=0)), reads=h2keys + [k_idx], writes=[kxg], dma=True)
                    for g0 in range(0, 32, 4):
                        tp, ktp = bank("tpE", (g0 // 4) % 2 + 6, 1, BF16)
                        for i in range(4):
                            kc = g0 + i
                            OP("pe", lambda e, tp=tp, i=i, kc=kc, xg=xg: e.transpose(tp[:, i * 128:(i + 1) * 128], xg[:, kc * 128:(kc + 1) * 128], ident_b),
                               [kxg, k_idb], [ktp])
                        dst = xgT.rearrange("p (k s) -> p k s", s=256)[:, g0:g0 + 4, jh * 128:(jh + 1) * 128]
                        OP("act", lambda e, tp=tp, dst=dst: e.copy(out=dst, in_=tp[:, 0:512].rearrange("p (k s) -> p k s", s=128)), [ktp], [k_xgT])
                hidT, k_hid = A.carve("hidT", 88 * K, 8192, BF16)
                for fc in range(16):
                    hps = []
                    for wi, wd in enumerate((w1_d, w3_d)):
                        st, kst = A.carve("wstE", 32 * K + ((fc * 2 + wi) % 2) * 16384, 16384)
                        wb, kwb = A.carve("wbE", 64 * K + ((fc * 2 + wi) % 3) * 8192, 8192, BF16)
                        for q4 in range(4):
                            stq, kstq = A.carve("wstEq", 32 * K + ((fc * 2 + wi) % 2) * 16384 + q4 * 4096, 4096)
                            S.op("sp", lambda e, stq=stq, wd=wd, q4=q4, fc=fc, ex=ex: e.dma_start(
                                out=stq.rearrange("p (k n) -> p k n", n=128),
                                in_=wd[ex, q4 * 1024:(q4 + 1) * 1024, fc * 128:(fc + 1) * 128].rearrange("(k p) n -> p k n", p=128)),
                                writes=[kstq], dma=True)
                            wbq, kwbq = A.carve("wbEq", 64 * K + ((fc * 2 + wi) % 3) * 8192 + q4 * 2048, 2048, BF16)
                            cast(wbq, stq, kstq, kwbq)
                            if q4 == 0:
                                qs = []
                            qs.append((wbq, kwbq))
                        hp, khp = bank("hps", (fc % 2) * 2 + wi)
                        for kc in range(32):
                            wbq, kwbq = qs[kc // 8]
                            OP("pe", lambda e, hp=hp, wbq=wbq, kc=kc: e.matmul(
                                hp[:, 0:256], wbq[:, (kc % 8) * 128:(kc % 8 + 1) * 128], xgT[:, kc * 256:(kc + 1) * 256],
                                start=(kc == 0), stop=(kc == 31)), [kwbq, k_xgT], [khp])
                        hps.append((hp, khp))
                    (h1, kh1), (h3, kh3) = hps
                    tm, ktm = A.carve("tmE", 96 * K + (fc % 2) * 1024, 1024)
                    OP("act", lambda e, tm=tm, h1=h1: e.activation(out=tm, in_=h1[:, 0:256], func=AF.Exp, scale=-1.0), [kh1], [ktm])
                    OP("dve", lambda e, tm=tm: e.tensor_scalar(out=tm, in0=tm, scalar1=1.0, scalar2=None, op0=ALU.add), [ktm], [ktm])
                    OP("dve", lambda e, tm=tm: e.reciprocal(out=tm, in_=tm), [ktm], [ktm])
                    OP("dve", lambda e, tm=tm, h1=h1: e.tensor_tensor(out=tm, in0=tm, in1=h1[:, 0:256], op=ALU.mult), [ktm, kh1], [ktm])
                    OP("dve", lambda e, tm=tm, h3=h3, fc=fc: e.tensor_tensor(out=hidT[:, fc * 256:(fc + 1) * 256], in0=tm, in1=h3[:, 0:256], op=ALU.mult),
                       [ktm, kh3], [k_hid])
                ybs = [A.carve("ybuf", 146 * K + jh * 8192, 8192) for jh in range(2)]
                for db in range(8):
                    w2q = []
                    for pc in range(2):
                        st, kst = A.carve("wst2", 98 * K + ((db * 2 + pc) % 2) * 16384, 16384)
                        S.op("sp", lambda e, st=st, pc=pc, db=db, ex=ex: e.dma_start(
                            out=st.rearrange("p (k n) -> p k n", n=512),
                            in_=w2_d[ex, pc * 1024:(pc + 1) * 1024, db * 512:(db + 1) * 512].rearrange("(k p) n -> p k n", p=128)),
                            writes=[kst], dma=True)
                        wb, kwb = A.carve("wb2", 130 * K + ((db * 2 + pc) % 2) * 8192, 8192, BF16)
                        cast(wb, st, kst, kwb)
                        w2q.append((wb, kwb))
                    for jh in range(2):
                        yp, kyp = bank("yps", 4 + jh)
                        for k16 in range(16):
                            wb, kwb = w2q[k16 // 8]
                            OP("pe", lambda e, yp=yp, wb=wb, k16=k16, jh=jh: e.matmul(
                                yp, hidT[:, k16 * 256 + jh * 128: k16 * 256 + (jh + 1) * 128], wb[:, (k16 % 8) * 512:(k16 % 8 + 1) * 512],
                                start=(k16 == 0), stop=(k16 == 15)), [k_hid, kwb], [kyp])
                        yb, kyb = ybs[jh]
                        col = ex * 2 + jh
                        OP("act", lambda e, yb=yb, yp=yp, db=db, col=col: e.activation(
                            out=yb[:, (db % 4) * 512:(db % 4 + 1) * 512], in_=yp, func=AF.Identity, scale=gate[:, col:col + 1]), [kyp, k_gate], [kyb])
                        if db % 4 == 3:
                            dh = db // 4
                            S.op("pool", lambda e, yb=yb, col=col, dh=dh: e.indirect_dma_start(
                                out=moe_d[dh][:, :], out_offset=_b.IndirectOffsetOnAxis(ap=idx_i[:, col:col + 1], axis=0),
                                in_=yb, in_offset=None, compute_op=ALU.add),
                                reads=[kyb, k_idx], writes=[("moe", dh)], dma=True)

        def phase_F():
            K = 1024
            G2b, k_G2b = A.carve("G2b", 96 * K, 16384)
            FNb, k_FNb = A.carve("FNb", 112 * K, 16384)
            g2col, k_g2c = A.carve("g2col", P2 + 2880 + 1024, 128)
            fncol, k_fnc = A.carve("fncol", P2 + 2880 + 1152, 128)
            OP("dve", lambda e: e.tensor_copy(out=g2col, in_=mods3[:, 160:192, 0]), [k_mods], [k_g2c])
            ld(fncol, fngT_d, k_fnc)
            bcast_row(g2col, k_g2c, G2b, k_G2b)
            bcast_row(fncol, k_fnc, FNb, k_FNb)
            for tt in range(16):
                x1t, kx1 = A.carve("x1tF", (tt % 2) * 16384, 16384)
                mo, kmo = A.carve("moF", 32 * K + (tt % 2) * 16384, 16384)
                jk, kjk = A.carve("jkF", 64 * K, 16384)
                S.op("sp", lambda e, x1t=x1t, tt=tt: e.dma_start(out=x1t, in_=x1_d[tt * 128:(tt + 1) * 128, :]),
                     reads=[("x1", tt)], writes=[kx1], dma=True)
                for dh in range(2):
                    S.op("sp", lambda e, mo=mo, tt=tt, dh=dh: e.dma_start(out=mo[:, dh * 2048:(dh + 1) * 2048], in_=moe_d[dh][tt * 128:(tt + 1) * 128, :]),
                         reads=[("moe", dh)], writes=[kmo], dma=True)
                OP("dve", lambda e, mo=mo: e.tensor_tensor(out=mo, in0=mo, in1=G2b, op=ALU.mult), [kmo, k_G2b], [kmo])
                OP("pool", lambda e, mo=mo, x1t=x1t: e.tensor_tensor(out=mo, in0=mo, in1=x1t, op=ALU.add), [kmo, kx1], [kmo])
                OP("pool", lambda e: e.memset(fss[:, 0:1], 0.0), [], [k_fss])
                OP("act", lambda e, jk=jk, mo=mo: e.activation(out=jk, in_=mo, func=AF.Square, accum_out=fss[:, 0:1]), [kmo, k_fss], [kjk, k_fss])
                OP("dve", lambda e: e.tensor_scalar(out=fss[:, 1:2], in0=fss[:, 0:1], scalar1=1.0 / D, scalar2=EPS, op0=ALU.mult, op1=ALU.add), [k_fss], [k_fss])
                OP("act", lambda e: e.activation(out=fss[:, 2:3], in_=fss[:, 1:2], func=AF.Sqrt), [k_fss], [k_fss])
                OP("dve", lambda e: e.reciprocal(out=fss[:, 3:4], in_=fss[:, 2:3]), [k_fss], [k_fss])
                OP("dve", lambda e, mo=mo, x1t=x1t: e.scalar_tensor_tensor(out=x1t, in0=mo, scalar=fss[:, 3:4], in1=FNb, op0=ALU.mult, op1=ALU.mult),
                   [kmo, k_fss, k_FNb], [kx1])
                S.op("act", lambda e, x1t=x1t, tt=tt: e.dma_start(out=out_d[tt * 128:(tt + 1) * 128, :], in_=x1t),
                     reads=[kx1], dma=True, is_out=True)

        if upto in ("p1", "A"):
            pass
        else:
            S.barrier()
            init_B_consts()
            S.barrier()
            try:
                phase_B()
            except _Stop:
                pass
            S.barrier()
            if upto != "B":
                phase_C()
                S.barrier()
                if upto != "C":
                    phase_D()
                    S.barrier()
                    if upto != "D":
                        phase_E()
                        S.barrier()
                        phase_F()

        if dbg and upto in ("C", "D"):
            dbg_out["x1"] = dout("d_x1", [L, D])
            for i in range(16):
                t, kt = A.carve("cp", (i % 2) * 16384, 16384)
                S.op("sp", lambda e, t=t, i=i: e.dma_start(out=t, in_=x1_d[i * 128:(i + 1) * 128, :]),
                     reads=[("x1", i)], writes=[kt], dma=True)
                S.op("sp", lambda e, t=t, i=i: e.dma_start(out=dbg_out["x1"][i * 128:(i + 1) * 128, :], in_=t),
                     reads=[kt], dma=True, is_out=True)
        if dbg and upto in ("D", "all"):
            dbg_out["afft"] = dout("d_afft", [128, 256])
            dbg_out["gate"] = dout("d_gate", [128, 32])
            dbg_out["idxf"] = dout("d_idxf", [128, 32])
            dbg_out["valT"] = dout("d_valT", [128, 256])
            S.op("sp", lambda e: e.dma_start(out=dbg_out["afft"], in_=afft_g[0]), reads=[afft_g[1]], dma=True, is_out=True)
            S.op("sp", lambda e: e.dma_start(out=dbg_out["valT"], in_=afft_g[2]), reads=[afft_g[3]], dma=True, is_out=True)
            S.op("sp", lambda e: e.dma_start(out=dbg_out["gate"], in_=gate), reads=[k_gate], dma=True, is_out=True)
            S.op("sp", lambda e: e.dma_start(out=dbg_out["idxf"], in_=idx_f), reads=[k_idxf], dma=True, is_out=True)
        if dbg and upto == "B":
            dbg_out["mixT"] = dout("d_mixT", [D, L], BF16)
            for i in range(32):
                t, kt = A.carve("cp", (i % 2) * 4096, 4096, BF16)
                S.op("sp", lambda e, t=t, i=i: e.dma_start(out=t, in_=mixT_d[i * 128:(i + 1) * 128, :]),
                     reads=[("mx", i)], writes=[kt], dma=True)
                S.op("sp", lambda e, t=t, i=i: e.dma_start(out=dbg_out["mixT"][i * 128:(i + 1) * 128, :], in_=t),
                     reads=[kt], dma=True, is_out=True)
        if dbg and upto == "A":
            dbg_out["projT"] = dout("d_projT", [INC, L])
            dbg_out["cprojT"] = dout("d_cprojT", [3 * HGW, CTX])
            for i in range(96):
                t, kt = A.carve("cp", 0, 8192)
                S.op("sp", lambda e, t=t, i=i: e.dma_start(out=t, in_=projT_d[i * 128:(i + 1) * 128, :]),
                     reads=[("pj", i)], writes=[kt], dma=True)
                S.op("sp", lambda e, t=t, i=i: e.dma_start(out=dbg_out["projT"][i * 128:(i + 1) * 128, :], in_=t),
                     reads=[kt], dma=True, is_out=True)
            for i in range(48):
                t, kt = A.carve("cp", 0, 1024)
                S.op("sp", lambda e, t=t, i=i: e.dma_start(out=t, in_=cprojT_d[i * 128:(i + 1) * 128, :]),
                     reads=[("cpj", i)], writes=[kt], dma=True)
                S.op("sp", lambda e, t=t, i=i: e.dma_start(out=dbg_out["cprojT"][i * 128:(i + 1) * 128, :], in_=t),
                     reads=[kt], dma=True, is_out=True)

        S.finish()
        S.emit(nc)
    return nc


def make_inputs(inp, b):
    f = lambda a: np.ascontiguousarray(a, dtype=np.float32)
    c2 = np.stack([inp["c"][b], inp["c_ctx"]], axis=1)
    c2T = c2.reshape(32, 128, 2).transpose(1, 0, 2).reshape(128, 64)
    colT = lambda v: np.ascontiguousarray(np.asarray(v).reshape(-1, 128).T)
    return {
        "x": f(inp["x"][b]),
        "ctx": f(inp["ctx"][b]),
        "c2T": f(c2T),
        "ada_w": f(inp["ada_w"][0]),
        "ada_bT": f(colT(inp["ada_b"][0])),
        "n1gT": f(colT(inp["norm1_g"][0])),
        "w_in": f(inp["w_in"][0]),
        "ident": np.eye(128, dtype=np.float32),
        "lbp": f(inp["lb_param"].reshape(2, 2, 16, 128).transpose(3, 0, 1, 2).reshape(128, 64)),
        "hgnT": f(colT(inp["hg_norm_g"][0])),
        "psclT": f(colT(inp["pool_scale"][0])),
        "mk": _consts()["mk"], "pmat": _consts()["pmat"], "icnt": _consts()["icnt"],
        "pool_w": f(inp["pool_w"][0]),
        "w_out": f(inp["w_out"][0]),
        "n2gT": f(colT(inp["norm2_g"][0])),
        "fngT": f(colT(inp["final_norm_g"])),
        "iota256": np.ascontiguousarray(np.broadcast_to(np.arange(1, 257, dtype=np.float32)[None, :], (128, 256))),
        "tokidx": np.ascontiguousarray((np.arange(16)[None, :] * 128 + np.arange(128)[:, None]).astype(np.float32)),
        "rwT": f(inp["router_w"][0].reshape(32, 128, 16).transpose(1, 0, 2).reshape(128, 512)),
        "moe_w1": f(inp["moe_w1"][0]), "moe_w3": f(inp["moe_w3"][0]), "moe_w2": f(inp["moe_w2"][0]),
    }


_CONST_CACHE = {}


def _consts():
    if _CONST_CACHE:
        return _CONST_CACHE
    p = np.arange(128)
    t = np.arange(64)
    mk = np.zeros((128, 128), np.float32)
    mk[:, 0:64] = ((p[:, None] % 64) <= t[None, :])
    mk[:, 64:128] = ((p[:, None] % 64) >= t[None, :])
    wins = (2, 4, 8, 16)
    offs = {0: (-1, 0), 1: (-1, 0, 1), 2: (-2, -1, 0, 1, 2), 3: (-4, -3, -2, -1, 0, 1, 2, 3, 4)}
    mats = []
    T = 8
    tl = np.arange(128)
    rt, ct = 2 * T + tl // 64, tl % 64
    for gi, w in enumerate(wins):
        for o in offs[gi]:
            rs, cs = 2 * (T + o) + tl // 64, tl % 64
            m = ((rs[:, None] >= rt[None, :] - w // 2) & (rs[:, None] <= rt[None, :] - w // 2 + w - 1) &
                 (cs[:, None] >= ct[None, :] - w // 2) & (cs[:, None] <= ct[None, :] - w // 2 + w - 1))
            mats.append(m.astype(np.float32))
    pmat = np.concatenate(mats, axis=1)
    icnt = np.zeros((4, 128, 2048), np.float32)
    for gi, w in enumerate(wins):
        def cntv(n):
            idx = np.arange(n)
            st = idx - w // 2
            return np.clip(st + w, 0, n) - np.clip(st, 0, n)
        cnt = (cntv(32)[:, None] * cntv(64)[None, :]).reshape(-1).astype(np.float32)
        icnt[gi] = (1.0 / cnt)[None, :]
    _CONST_CACHE.update(mk=mk, pmat=np.ascontiguousarray(pmat), icnt=icnt)
    return _CONST_CACHE


def kernel(**inputs):
    inp = {k: np.asarray(v) for k, v in inputs.items()}
    nc = build_program(upto="all", dbg=False)
    in_maps = [make_inputs(inp, b) for b in range(N_CORES)]
    res = run_bass_kernel_spmd(nc, in_maps, core_ids=list(range(N_CORES)))
    out = np.stack([np.asarray(r["out"]) for r in res.results], axis=0)
    return out.astype(np.float32)
```
